# Optimizing a Trainium2 kernel written in Bass

```python
import jax, jax.numpy as jnp
from jax import lax
import numpy as np

D_MODEL = 1024
BATCH = 4
SEQ = 8192
DEPTH = 2

N_META = 16
BLOCK_Q = 128
HEAD_DIM = 128
FOX_HEADS = 4
SB_HEADS = 4
FOX_WIDTH = FOX_HEADS * HEAD_DIM
SB_WIDTH = SB_HEADS * HEAD_DIM
LRU_WIDTH = 512
LRU_BLOCKS = 8
LRU_BLOCK_DIM = LRU_WIDTH // LRU_BLOCKS
LRU_C = 8.0
CONV_WIDTH = 4
N_BRANCH = 3
D_FF = 2816
N_EXPERTS = 8
TOP_K = 2
D_FF_EXPERT = 3584
N_DENSE = (DEPTH + 1) // 2
N_MOE = DEPTH // 2
RMS_EPS = 1e-6
NEG = -1e30
SPLIT_SIZES = (3 * FOX_WIDTH, FOX_HEADS, 3 * SB_WIDTH, LRU_WIDTH, LRU_WIDTH, N_BRANCH * D_MODEL)
IN_COLS = 3 * FOX_WIDTH + FOX_HEADS + 3 * SB_WIDTH + 2 * LRU_WIDTH + N_BRANCH * D_MODEL

kernel_name = 'hybrid_fox_stickbreak_rglru_moe'


def rms_norm(x, g):
    xf = x.astype(jnp.float32)
    y = xf * lax.rsqrt(jnp.mean(xf * xf, axis=-1, keepdims=True) + RMS_EPS)
    return (y * g.astype(jnp.float32)).astype(x.dtype)


def _to_blocks(t, nblk):
    b, h, _ = t.shape[:3]
    t = t.reshape((b, h, nblk, BLOCK_Q) + t.shape[3:])
    return jnp.moveaxis(t, 2, 0)


def _from_blocks(o):
    nblk, b, h, bq, dh = o.shape
    return o.transpose(1, 0, 3, 2, 4).reshape(b, nblk * bq, h * dh)


def _front_pad(t, pad):
    return jnp.pad(t, ((0, 0), (pad, 0)) + ((0, 0),) * (t.ndim - 2))


def forgetting_attention(q, k, v, log_f):
    b, l, h, dh = q.shape
    pad = (-l) % BLOCK_Q
    lp = l + pad
    nblk = lp // BLOCK_Q
    c = jnp.cumsum(log_f.astype(jnp.float32), axis=1)
    q, k, v = [_front_pad(t, pad).transpose(0, 2, 1, 3) for t in (q, k, v)]
    c = _front_pad(c, pad).transpose(0, 2, 1)
    kpos = jnp.arange(lp)
    scale = dh ** -0.5

    def block(args):
        qi, ci, i = args
        qpos = i * BLOCK_Q + jnp.arange(BLOCK_Q)
        s = (jnp.einsum('bhqd,bhkd->bhqk', qi, k).astype(jnp.float32) * scale
             + ci[..., None] - c[:, :, None, :])
        mask = (kpos[None, :] <= qpos[:, None]) & (kpos[None, :] >= pad)
        p = jax.nn.softmax(jnp.where(mask, s, NEG), axis=-1)
        return jnp.einsum('bhqk,bhkd->bhqd', p.astype(v.dtype), v)

    o = lax.map(block, (_to_blocks(q, nblk), _to_blocks(c, nblk), jnp.arange(nblk)))
    return _from_blocks(o)[:, pad:]


def stick_breaking_attention(q, k, v):
    b, l, h, dh = q.shape
    pad = (-l) % BLOCK_Q
    lp = l + pad
    nblk = lp // BLOCK_Q
    q, k, v = [_front_pad(t, pad).transpose(0, 2, 1, 3) for t in (q, k, v)]
    kpos = jnp.arange(lp)
    scale = dh ** -0.5

    def block(args):
        qi, i = args
        qpos = i * BLOCK_Q + jnp.arange(BLOCK_Q)
        z = jnp.einsum('bhqd,bhkd->bhqk', qi, k).astype(jnp.float32) * scale
        mask = (kpos[None, :] < qpos[:, None]) & (kpos[None, :] >= pad)
        m = jnp.where(mask, jax.nn.log_sigmoid(-z), 0.0)
        later = lax.cumsum(m, axis=3, reverse=True) - m
        a = jnp.where(mask, jnp.exp(jax.nn.log_sigmoid(z) + later), 0.0)
        return jnp.einsum('bhqk,bhkd->bhqd', a.astype(v.dtype), v)

    o = lax.map(block, (_to_blocks(q, nblk), jnp.arange(nblk)))
    return _from_blocks(o)[:, pad:]


def recurrent_branch(x_in, x_gate, conv_w, conv_b, wa, ba, wx, bx, lam):
    b, l, w = x_in.shape
    u = lax.conv_general_dilated(x_in, conv_w[:, None, :].astype(x_in.dtype), window_strides=(1,),
                                 padding=[(CONV_WIDTH - 1, 0)],
                                 dimension_numbers=('NWC', 'WIO', 'NWC'),
                                 feature_group_count=w) + conv_b
    ub = u.reshape(b, l, LRU_BLOCKS, LRU_BLOCK_DIM)
    r = jax.nn.sigmoid(jnp.einsum('blni,nij->blnj', ub, wa).reshape(b, l, w) + ba)
    gi = jax.nn.sigmoid(jnp.einsum('blni,nij->blnj', ub, wx).reshape(b, l, w) + bx)
    log_a = -LRU_C * r.astype(jnp.float32) * jax.nn.softplus(-lam.astype(jnp.float32))
    a = jnp.exp(log_a)
    inp = jnp.sqrt(-jnp.expm1(2.0 * log_a)) * (gi * u).astype(jnp.float32)

    def combine(e1, e2):
        a1, b1 = e1
        a2, b2 = e2
        return a1 * a2, a2 * b1 + b2

    _, hseq = lax.associative_scan(combine, (a, inp), axis=1)
    return hseq.astype(x_in.dtype) * jax.nn.gelu(x_gate)


def mixer(xn, w_in, b_forget, b_gate, conv_w, conv_b, wa, ba, wx, bx, lam,
          w_fox_o, w_sb_o, w_lru_o, w_out):
    b, l, d = xn.shape
    proj = xn @ w_in
    idx = [int(v) for v in np.cumsum(SPLIT_SIZES)[:-1]]
    fox_qkv, fox_f, sb_qkv, lru_x, lru_g, gates = jnp.split(proj, idx, axis=-1)
    fox_qkv = fox_qkv.reshape(b, l, 3, FOX_HEADS, HEAD_DIM)
    sb_qkv = sb_qkv.reshape(b, l, 3, SB_HEADS, HEAD_DIM)
    log_f = jax.nn.log_sigmoid((fox_f + b_forget).astype(jnp.float32))
    y_fox = forgetting_attention(fox_qkv[:, :, 0], fox_qkv[:, :, 1], fox_qkv[:, :, 2], log_f) @ w_fox_o
    y_sb = stick_breaking_attention(sb_qkv[:, :, 0], sb_qkv[:, :, 1], sb_qkv[:, :, 2]) @ w_sb_o
    y_lru = recurrent_branch(lru_x, lru_g, conv_w, conv_b, wa, ba, wx, bx, lam) @ w_lru_o
    g = jax.nn.sigmoid(gates + b_gate).reshape(b, l, N_BRANCH, d)
    merged = g[:, :, 0] * y_fox + g[:, :, 1] * y_sb + g[:, :, 2] * y_lru
    return merged @ w_out


def swiglu(x, wg, wu, wd):
    return (jax.nn.silu(x @ wg) * (x @ wu)) @ wd


def moe_swiglu(x, router_w, wg, wu, wd):
    b, l, d = x.shape
    xt = x.reshape(-1, d)
    logits = (xt @ router_w).astype(jnp.float32)
    top_v, top_i = lax.top_k(logits, TOP_K)
    top_w = jax.nn.softmax(top_v, axis=-1)
    comb = jnp.sum(jax.nn.one_hot(top_i, N_EXPERTS, dtype=jnp.float32) * top_w[..., None], axis=1)
    comb = comb.astype(x.dtype)
    out = jnp.zeros_like(xt)
    for e in range(N_EXPERTS):
        out = out + comb[:, e:e + 1] * swiglu(xt, wg[e], wu[e], wd[e])
    return out.reshape(b, l, d)


def setup_inputs(seed: int = 0) -> dict:
    key = jax.random.key(seed)
    ks = iter(jax.random.split(key, 40))
    f32 = jnp.float32

    def nrm(shape, scale):
        return jax.random.normal(next(ks), shape, f32) * scale

    u = jax.random.uniform(next(ks), (DEPTH, LRU_WIDTH), f32, 0.9, 0.999)
    a_base = u ** (1.0 / LRU_C)
    lru_lambda = jnp.log(a_base) - jnp.log1p(-a_base)
    return {
        'x': nrm((BATCH, SEQ, D_MODEL), 1.0),
        'meta_tokens': nrm((N_META, D_MODEL), 1.0),
        'g_mix': 1.0 + nrm((DEPTH, D_MODEL), 0.02),
        'w_in': nrm((DEPTH, D_MODEL, IN_COLS), D_MODEL ** -0.5),
        'b_forget': 3.0 + nrm((DEPTH, FOX_HEADS), 0.5),
        'b_gate': nrm((DEPTH, N_BRANCH * D_MODEL), 0.02),
        'conv_w': nrm((DEPTH, CONV_WIDTH, LRU_WIDTH), CONV_WIDTH ** -0.5),
        'conv_b': nrm((DEPTH, LRU_WIDTH), 0.02),
        'lru_wa': nrm((DEPTH, LRU_BLOCKS, LRU_BLOCK_DIM, LRU_BLOCK_DIM), LRU_BLOCK_DIM ** -0.5),
        'lru_ba': nrm((DEPTH, LRU_WIDTH), 0.02),
        'lru_wx': nrm((DEPTH, LRU_BLOCKS, LRU_BLOCK_DIM, LRU_BLOCK_DIM), LRU_BLOCK_DIM ** -0.5),
        'lru_bx': nrm((DEPTH, LRU_WIDTH), 0.02),
        'lru_lambda': lru_lambda,
        'w_fox_o': nrm((DEPTH, FOX_WIDTH, D_MODEL), FOX_WIDTH ** -0.5),
        'w_sb_o': nrm((DEPTH, SB_WIDTH, D_MODEL), SB_WIDTH ** -0.5),
        'w_lru_o': nrm((DEPTH, LRU_WIDTH, D_MODEL), LRU_WIDTH ** -0.5),
        'w_out': nrm((DEPTH, D_MODEL, D_MODEL), D_MODEL ** -0.5),
        'g_ffn': 1.0 + nrm((DEPTH, D_MODEL), 0.02),
        'ffn_w_gate': nrm((N_DENSE, D_MODEL, D_FF), D_MODEL ** -0.5),
        'ffn_w_up': nrm((N_DENSE, D_MODEL, D_FF), D_MODEL ** -0.5),
        'ffn_w_down': nrm((N_DENSE, D_FF, D_MODEL), D_FF ** -0.5),
        'router_w': nrm((N_MOE, D_MODEL, N_EXPERTS), D_MODEL ** -0.5),
        'moe_w_gate': nrm((N_MOE, N_EXPERTS, D_MODEL, D_FF_EXPERT), D_MODEL ** -0.5),
        'moe_w_up': nrm((N_MOE, N_EXPERTS, D_MODEL, D_FF_EXPERT), D_MODEL ** -0.5),
        'moe_w_down': nrm((N_MOE, N_EXPERTS, D_FF_EXPERT, D_MODEL), D_FF_EXPERT ** -0.5),
        'g_final': 1.0 + nrm((D_MODEL,), 0.02),
    }


def reference(x, meta_tokens, g_mix, w_in, b_forget, b_gate, conv_w, conv_b, lru_wa, lru_ba,
              lru_wx, lru_bx, lru_lambda, w_fox_o, w_sb_o, w_lru_o, w_out, g_ffn,
              ffn_w_gate, ffn_w_up, ffn_w_down, router_w, moe_w_gate, moe_w_up, moe_w_down,
              g_final):
    b = x.shape[0]
    meta = jnp.broadcast_to(meta_tokens[None].astype(x.dtype), (b, N_META, x.shape[-1]))
    h = jnp.concatenate([meta, x], axis=1)
    for layer in range(DEPTH):
        h = h + mixer(rms_norm(h, g_mix[layer]), w_in[layer], b_forget[layer], b_gate[layer],
                      conv_w[layer], conv_b[layer], lru_wa[layer], lru_ba[layer],
                      lru_wx[layer], lru_bx[layer], lru_lambda[layer],
                      w_fox_o[layer], w_sb_o[layer], w_lru_o[layer], w_out[layer])
        hn = rms_norm(h, g_ffn[layer])
        j = layer // 2
        if layer % 2 == 0:
            h = h + swiglu(hn, ffn_w_gate[j], ffn_w_up[j], ffn_w_down[j])
        else:
            h = h + moe_swiglu(hn, router_w[j], moe_w_gate[j], moe_w_up[j], moe_w_down[j])
    return rms_norm(h, g_final)[:, N_META:]
```

```python
import numpy as np
import concourse.bass as bass
import concourse.mybir as mybir
from concourse.bass_utils import run_bass_kernel_spmd

F32 = mybir.dt.float32
BF16 = mybir.dt.bfloat16
ALU = mybir.AluOpType
AF = mybir.ActivationFunctionType

ENGS = ("pe", "act", "dve", "pool", "sp")
EPOCH = 12000
N_DMA_SLOTS = 12
SAME_ENGINE_SYNC = True
ATTN_INTERLEAVE = True


class TT:
    def __init__(self, h, name):
        self.h = h
        self.name = name
        self.st = {}

    def __getitem__(self, idx):
        return self.h[idx]


class Prog:
    def __init__(self, nc):
        self.nc = nc
        self.ops = {e: [] for e in ENGS}
        self.cnt = {e: 0 for e in ENGS}
        self.seen = {e: {} for e in ENGS}
        self.dma_slot_uses = {}
        self.dma_rr = {e: 0 for e in ENGS}
        self.max_epoch = {e: 0 for e in ENGS}

    def _state(self, t, key):
        st = t.st.get(key)
        if st is None:
            st = {"w": None, "r": {}}
            t.st[key] = st
        return st

    def _collect(self, eng, reads, writes):
        need = {}

        def req(tok):
            if tok is None:
                return
            k, v = tok
            if k[0] == "e" and k[1] == eng and (eng == "pe" or not SAME_ENGINE_SYNC):
                return
            if k[0] == "e" and k[1] == eng and eng == "sp":
                return
            if need.get(k, 0) < v:
                need[k] = v

        for (t, key) in reads:
            req(self._state(t, key)["w"])
        for (t, key) in writes:
            st = self._state(t, key)
            req(st["w"])
            for tok in st["r"].values():
                req(tok)
        waits = []
        seen = self.seen[eng]
        for k, v in need.items():
            if seen.get(k, 0) >= v:
                continue
            seen[k] = v
            waits.append((k, v))
        return waits

    def _commit(self, tok, reads, writes):
        for (t, key) in reads:
            st = self._state(t, key)
            st["r"][tok[0]] = tok
        for (t, key) in writes:
            st = self._state(t, key)
            st["w"] = tok
            st["r"] = {}

    def op(self, eng, fn, reads=(), writes=()):
        waits = self._collect(eng, reads, writes)
        self.cnt[eng] += 1
        seq = self.cnt[eng]
        epoch = (seq - 1) // EPOCH
        self.max_epoch[eng] = max(self.max_epoch[eng], epoch)
        tok = (("e", eng, epoch), seq - epoch * EPOCH)
        self.ops[eng].append((waits, fn, tok[0], 1))
        self._commit(tok, reads, writes)
        return tok

    def dma(self, q, fn, reads=(), writes=()):
        slot = self.dma_rr[q] % N_DMA_SLOTS
        self.dma_rr[q] += 1
        k = ("d", q, slot)
        uses = self.dma_slot_uses.get(k, 0)
        waits = self._collect(q, reads, writes)
        if uses > 0 and self.seen[q].get(k, 0) < 16 * uses:
            self.seen[q][k] = 16 * uses
            waits.append((k, 16 * uses))
        uses += 1
        self.dma_slot_uses[k] = uses
        tok = (k, 16 * uses)
        self.ops[q].append((waits, fn, k, 16))
        self._commit(tok, reads, writes)
        return tok

    def _final_tokens(self):
        toks = []
        for e in ENGS:
            if e == "sp" or self.cnt[e] == 0:
                continue
            seq = self.cnt[e]
            epoch = (seq - 1) // EPOCH
            toks.append((("e", e, epoch), seq - epoch * EPOCH))
        for k, uses in self.dma_slot_uses.items():
            toks.append((k, 16 * uses))
        return toks

    def barrier(self):
        toks = self._final_tokens()
        for e in ENGS:
            self.wait_all(e, [t for t in toks if not (t[0][0] == "e" and t[0][1] == e)])

    def final_wait(self, eng="sp"):
        self.wait_all(eng, [t for t in self._final_tokens() if t[0][0] == "d"])

    def wait_all(self, eng, toks):
        waits = []
        for tok in toks:
            k, v = tok
            if self.seen[eng].get(k, 0) < v:
                self.seen[eng][k] = v
                waits.append((k, v))
        self.ops[eng].append((waits, None, None, 0))

    def build(self, stack):
        nc = self.nc
        sems = {}
        for e in ENGS:
            if e == "sp":
                continue
            for ep in range(self.max_epoch[e] + 1):
                sems[("e", e, ep)] = stack.enter_context(nc.semaphore(f"s_{e}_{ep}"))
        for k in self.dma_slot_uses:
            sems[k] = stack.enter_context(nc.semaphore(f"d_{k[1]}_{k[2]}"))
        block = stack.enter_context(nc.Block())
        ops = self.ops

        def run(engobj, lst):
            for waits, fn, semk, inc in lst:
                for (k, v) in waits:
                    engobj.wait_ge(sems[k], v)
                if fn is not None:
                    inst = fn(engobj)
                    inst.then_inc(sems[semk], inc)

        @block.tensor
        def _(e):
            run(e, ops["pe"])

        @block.scalar
        def _(e):
            run(e, ops["act"])

        @block.vector
        def _(e):
            run(e, ops["dve"])

        @block.gpsimd
        def _(e):
            run(e, ops["pool"])

        @block.sync
        def _(e):
            run(e, ops["sp"])


D = 1024
KC = 8
RMS_EPS = 1e-6
QSCALE = 128.0 ** -0.5
GELU_K = 2.0 * 0.7978845608028654


ARENA_WORDS = 52000


class Ctx:
    def __init__(self, nc, stack):
        self.nc = nc
        self.st = stack
        self.p = Prog(nc)
        self.banks = [TT(stack.enter_context(nc.psum_tensor(f"bank{i}", [128, 512], F32)), f"bank{i}")
                      for i in range(8)]
        self.arena = stack.enter_context(nc.sbuf_tensor("arena", [128, ARENA_WORDS], F32))
        self.off = 0

    def sb(self, name, shape, dt=F32):
        n = 1
        for d_ in shape[1:]:
            n *= d_
        esz = 4 if dt == F32 else 2
        words = (n * esz + 3) // 4
        words = (words + 7) // 8 * 8
        assert self.off + words <= ARENA_WORDS, f"arena overflow allocating {name}: {self.off}+{words}"
        v = self.arena[0:shape[0], self.off:self.off + words]
        self.off += words
        if dt != F32:
            v = v.bitcast(dt)
        v = v[:, 0:n]
        if len(shape) == 3:
            v = v.rearrange("p (a b) -> p a b", a=shape[1])
        elif len(shape) == 4:
            v = v.rearrange("p (a b c) -> p a b c", a=shape[1], b=shape[2])
        return TT(v, name)

    def mark(self):
        return self.off

    def reset(self, mark):
        self.p.barrier()
        self.off = mark

    def dram(self, name, shape, dt, kind="Internal"):
        if kind == "Internal":
            return TT(self.nc.dram_tensor(name, shape, dt), name)
        return TT(self.nc.dram_tensor(name, shape, dt, kind=kind), name)


def R(*tts):
    return [(t, 0) for t in tts]


def emit_rmsnorm(cx, ht, w, g_sb, ones_bf, sq, ssbank, lnv, rstd, xn, xn32=None):
    p = cx.p
    p.op("act", lambda e: e.activation(out=sq[:, :, :w], in_=ht[:, :, :w], func=AF.Square),
         reads=R(ht), writes=R(sq))
    for c in range(KC):
        p.op("pe", lambda e, c=c: e.matmul(ssbank[:, :w], ones_bf[:, :], sq[:, c, :w], start=(c == 0), stop=(c == KC - 1)),
             reads=R(ones_bf, sq), writes=R(ssbank))
    p.op("act", lambda e: e.activation(out=lnv[:, :w], in_=ssbank[:, :w], func=AF.Ln, scale=1.0 / D, bias=RMS_EPS),
         reads=R(ssbank), writes=R(lnv))
    p.op("act", lambda e: e.activation(out=rstd[:, :w], in_=lnv[:, :w], func=AF.Exp, scale=-0.5),
         reads=R(lnv), writes=R(rstd))
    for c in range(KC):
        p.op("dve", lambda e, c=c: e.scalar_tensor_tensor(out=xn[:, c, :w], in0=ht[:, c, :w], scalar=g_sb[:, c:c + 1],
                                                            in1=rstd[:, :w], op0=ALU.mult, op1=ALU.mult),
             reads=R(ht, g_sb, rstd), writes=R(xn))
        if xn32 is not None:
            p.op("dve", lambda e, c=c: e.scalar_tensor_tensor(out=xn32[:, c, :w], in0=ht[:, c, :w], scalar=g_sb[:, c:c + 1],
                                                                 in1=rstd[:, :w], op0=ALU.mult, op1=ALU.mult),
                 reads=R(ht, g_sb, rstd), writes=R(xn32))


def emit_proj(cx, bank, w_sb, col0, ncols, xn, w, reads_extra=()):
    p = cx.p
    for c in range(KC):
        p.op("pe", lambda e, c=c: e.matmul(bank[:ncols, :w], w_sb[:, c, col0:col0 + ncols], xn[:, c, :w],
                                           start=(c == 0), stop=(c == KC - 1)),
             reads=R(w_sb, xn), writes=R(bank))


def stage_A(cx, Lp, I, hT, hT_t, attT, attT_t, rows, S):
    p = cx.p
    bk = cx.banks
    NB = Lp // 128
    tiles = [(t0, min(512, Lp - t0)) for t0 in range(0, Lp, 512)]
    QK, Vd, CB = S["QK"], S["Vd"], S["CB"]
    ones_bf, negu, mle, mlt = S["ones_bf"], S["negu"], S["mle"], S["mlt"]
    gmix_d, wqk_d, wv_d, wf_d, wl_d = I["gmix"], I["w_qk"], I["w_v"], I["w_f"], I["w_lru"]
    nbf_d, convw_d, lvec_d, wab_d = I["nb_f"], I["conv_w"], I["lvec"], I["wab"]
    cx.reset(S["mark0"])
    if True:
        gmix = cx.sb("gmix_sb", [128, 8])
        p.dma("sp", lambda e: e.dma_start(out=gmix[:], in_=gmix_d), writes=R(gmix))
        mark1 = cx.mark()
        wqk = cx.sb("wqk", [128, 8, 1024], BF16)
        wv = cx.sb("wv", [128, 8, 512], BF16)
        wl = cx.sb("wl", [128, 8, 512], BF16)
        wf = cx.sb("wf", [128, 8, 2], BF16)
        for c in range(KC):
            p.dma("pool", lambda e, c=c: e.dma_start(out=wqk[:, c, :], in_=wqk_d[c * 128:(c + 1) * 128, :]), writes=R(wqk))
        for c in range(KC):
            p.dma("pool", lambda e, c=c: e.dma_start(out=wv[:, c, :], in_=wv_d[c * 128:(c + 1) * 128, :]), writes=R(wv))
            p.dma("pool", lambda e, c=c: e.dma_start(out=wl[:, c, :], in_=wl_d[c * 128:(c + 1) * 128, :]), writes=R(wl))
            p.dma("pool", lambda e, c=c: e.dma_start(out=wf[:, c, :], in_=wf_d[c * 128:(c + 1) * 128, :]), writes=R(wf))
        nbf = cx.sb("nbf", [2, 1])
        p.dma("sp", lambda e: e.dma_start(out=nbf[:], in_=nbf_d), writes=R(nbf))
        convw = cx.sb("convw", [128, 2, 4])
        lvec = cx.sb("lvec", [128, 2, 4])
        p.dma("sp", lambda e: e.dma_start(out=convw[:], in_=convw_d), writes=R(convw))
        p.dma("sp", lambda e: e.dma_start(out=lvec[:], in_=lvec_d), writes=R(lvec))
        wab = cx.sb("wab", [128, 2, 256], BF16)
        p.dma("pool", lambda e: e.dma_start(out=wab[:], in_=wab_d), writes=R(wab))
        le = cx.sb("le", [128, 2])
        sneg = cx.sb("sneg", [128, 2])
        sneg2 = cx.sb("sneg2", [128, 2])
        p.op("act", lambda e: e.activation(out=le[:], in_=lvec[:, :, 3], func=AF.Exp, scale=-1.0), reads=R(lvec), writes=R(le))
        p.op("act", lambda e: e.activation(out=le[:], in_=le[:], func=AF.Ln, bias=1.0), reads=R(le), writes=R(le))
        p.op("dve", lambda e: e.tensor_scalar(out=sneg[:], in0=le[:], scalar1=-8.0, scalar2=None, op0=ALU.mult), reads=R(le), writes=R(sneg))
        p.op("dve", lambda e: e.tensor_scalar(out=sneg2[:], in0=le[:], scalar1=-16.0, scalar2=None, op0=ALU.mult), reads=R(le), writes=R(sneg2))

        htb = [cx.sb(f"ht{i}", [128, 8, 512]) for i in range(2)]
        xnb = [cx.sb(f"xn{i}", [128, 8, 512], BF16) for i in range(2)]
        sq = cx.sb("sq", [128, 8, 512], BF16)
        lnv = cx.sb("lnv", [128, 512])
        rstd = cx.sb("rstd", [128, 512])
        qkst = [cx.sb(f"qkst{i}", [128, 8, 512], BF16) for i in range(2)]
        vst = [cx.sb(f"vst{i}", [128, 4, 512], BF16) for i in range(2)]
        ones2 = cx.sb("ones2", [2, 512])
        p.op("pool", lambda e: e.memset(ones2[:], 1.0), writes=R(ones2))
        fe = cx.sb("fe", [2, 512])
        fl = cx.sb("fl", [2, 512])
        cbuf = [cx.sb(f"cbuf{i}", [2, 512]) for i in range(2)]
        cr1 = cx.sb("cr1", [2, 512])
        cr2 = cx.sb("cr2", [2, 512])
        c3 = [cx.sb(f"c3_{i}", [2, 3, 512], BF16) for i in range(2)]
        xbuf = [cx.sb(f"xbuf{cc}", [128, 3 + 512]) for cc in range(2)]
        for cc in range(2):
            p.op("pool", lambda e, cc=cc: e.memset(xbuf[cc][:], 0.0), writes=R(xbuf[cc]))
        xg = cx.sb("xg", [128, 512])
        u = cx.sb("u", [128, 512])
        ub = cx.sb("ub", [128, 512], BF16)
        r_ = cx.sb("r_", [128, 512])
        gi = cx.sb("gi", [128, 512])
        a_ = cx.sb("a_", [128, 512])
        a2 = cx.sb("a2", [128, 512])
        ml = cx.sb("ml", [128, 512])
        inp = cx.sb("inp", [128, 512])
        hbuf = [[cx.sb(f"hbuf{cc}_{i}", [128, 512]) for i in range(2)] for cc in range(2)]
        gx = cx.sb("gx", [128, 512])
        sg = cx.sb("sg", [128, 512])
        lost = [cx.sb(f"lost{i}", [128, 512], BF16) for i in range(2)]

        rot = [bk[1], bk[2], bk[3], bk[7]]
        rc = [0]

        def nextbank():
            b = rot[rc[0] % 4]
            rc[0] += 1
            return b

        ev = [0]

        def evac(dst_ap_fn, dst_tt, bank, nrow, w, scale=None):
            if scale is not None or ev[0] % 2 == 0:
                sc = 1.0 if scale is None else scale
                p.op("act", lambda e: e.activation(out=dst_ap_fn(), in_=bank[:nrow, :w], func=AF.Copy, scale=sc),
                     reads=R(bank), writes=R(dst_tt))
            else:
                p.op("dve", lambda e: e.tensor_copy(out=dst_ap_fn(), in_=bank[:nrow, :w]), reads=R(bank), writes=R(dst_tt))
            ev[0] += 1

        wprev = None
        for i, (t0, w) in enumerate(tiles):
            ht = htb[i % 2]
            xn = xnb[i % 2]
            p.dma("sp", lambda e, ht=ht, t0=t0, w=w: e.dma_start(
                out=ht[:, :, :w], in_=hT.rearrange("(c p) t -> p c t", p=128)[:, :, t0:t0 + w]), reads=R(hT_t), writes=R(ht))
            emit_rmsnorm(cx, ht, w, gmix, ones_bf, sq, bk[0], lnv, rstd, xn)
            qs = qkst[i % 2]
            for m in range(8):
                b = nextbank()
                emit_proj(cx, b, wqk, m * 128, 128, xn, w)
                evac(lambda qs=qs, m=m, w=w: qs[:, m, :w], qs, b, 128, w, scale=(QSCALE if m in (0, 1, 4, 5) else None))
            p.dma("sp", lambda e, qs=qs, t0=t0, w=w: e.dma_start(
                out=QK.h.ap().rearrange("m p t -> p m t")[:, :, t0:t0 + w], in_=qs[:, :, :w]), reads=R(qs), writes=[(QK, ("w", t0))])
            vs = vst[i % 2]
            ns = w // 128
            for s in range(ns):
                b = nextbank()
                for c in range(KC):
                    p.op("pe", lambda e, c=c, s=s, b=b, xn=xn: e.matmul(b[:, :], xn[:, c, s * 128:(s + 1) * 128], wv[:, c, :],
                                                                     start=(c == 0), stop=(c == KC - 1)),
                         reads=R(xn, wv), writes=R(b))
                evac(lambda vs=vs, s=s: vs[:, s, :], vs, b, 128, 512)
            for h in range(4):
                p.dma("sp", lambda e, vs=vs, h=h, t0=t0, ns=ns: e.dma_start(
                    out=Vd.h.ap()[h, :, t0 // 128:t0 // 128 + ns, :], in_=vs[:, :ns, h * 128:(h + 1) * 128]),
                    reads=R(vs), writes=[(Vd, ("w", h, t0))])
            emit_proj(cx, bk[4], wf, 0, 2, xn, w)
            p.op("act", lambda e, w=w: e.activation(out=fe[:, :w], in_=bk[4][:2, :w], func=AF.Exp, scale=-1.0, bias=nbf[:, 0:1]),
                 reads=R(bk[4], nbf), writes=R(fe))
            p.op("act", lambda e, w=w: e.activation(out=fl[:, :w], in_=fe[:, :w], func=AF.Ln, bias=1.0), reads=R(fe), writes=R(fl))
            cb_ = cbuf[i % 2]
            if i == 0:
                p.op("dve", lambda e, cb_=cb_, w=w: e.tensor_tensor_scan(cb_[:, :w], ones2[:, :w], fl[:, :w], 0.0, ALU.mult, ALU.subtract),
                     reads=R(ones2, fl), writes=R(cb_))
            else:
                cprev = cbuf[(i - 1) % 2]
                p.op("dve", lambda e, cb_=cb_, w=w, cprev=cprev, wprev=wprev: e.tensor_tensor_scan(
                    cb_[:, :w], ones2[:, :w], fl[:, :w], cprev[:, wprev - 1:wprev], ALU.mult, ALU.subtract),
                    reads=R(ones2, fl, cprev), writes=R(cb_))
            c3t = c3[i % 2]
            p.op("dve", lambda e, c3t=c3t, cb_=cb_, w=w: e.tensor_copy(out=c3t[:, 0, :w], in_=cb_[:, :w]), reads=R(cb_), writes=R(c3t))
            p.op("dve", lambda e, c3t=c3t, cb_=cb_, w=w: e.tensor_tensor(out=cr1[:, :w], in0=cb_[:, :w], in1=c3t[:, 0, :w], op=ALU.subtract),
                 reads=R(cb_, c3t), writes=R(cr1))
            p.op("dve", lambda e, c3t=c3t, w=w: e.tensor_copy(out=c3t[:, 1, :w], in_=cr1[:, :w]), reads=R(cr1), writes=R(c3t))
            p.op("dve", lambda e, c3t=c3t, w=w: e.tensor_tensor(out=cr2[:, :w], in0=cr1[:, :w], in1=c3t[:, 1, :w], op=ALU.subtract),
                 reads=R(cr1, c3t), writes=R(cr2))
            p.op("dve", lambda e, c3t=c3t, w=w: e.tensor_copy(out=c3t[:, 2, :w], in_=cr2[:, :w]), reads=R(cr2), writes=R(c3t))
            p.dma("sp", lambda e, c3t=c3t, t0=t0, w=w: e.dma_start(out=CB.h.ap()[:, :, t0:t0 + w], in_=c3t[:, :, :w]),
                  reads=R(c3t), writes=[(CB, ("w", t0))])
            for cc in range(2):
                xb_ = xbuf[cc]
                bx_ = nextbank()
                emit_proj(cx, bx_, wl, cc * 128, 128, xn, w)
                bg_ = nextbank()
                emit_proj(cx, bg_, wl, 256 + cc * 128, 128, xn, w)
                if i > 0:
                    p.op("dve", lambda e, xb_=xb_, wprev=wprev: e.tensor_copy(out=xb_[:, 0:3], in_=xb_[:, wprev:wprev + 3]),
                         reads=R(xb_), writes=R(xb_))
                p.op("act", lambda e, xb_=xb_, bx_=bx_, w=w: e.activation(out=xb_[:, 3:3 + w], in_=bx_[:, :w], func=AF.Copy),
                     reads=R(bx_), writes=R(xb_))
                p.op("act", lambda e, bg_=bg_, w=w: e.activation(out=xg[:, :w], in_=bg_[:, :w], func=AF.Copy),
                     reads=R(bg_), writes=R(xg))
                p.op("dve", lambda e, xb_=xb_, cc=cc, w=w: e.tensor_scalar(out=u[:, :w], in0=xb_[:, 3:3 + w], scalar1=convw[:, cc, 3:4],
                                                                      scalar2=lvec[:, cc, 0:1], op0=ALU.mult, op1=ALU.add),
                     reads=R(xb_, convw, lvec), writes=R(u))
                for j in (2, 1, 0):
                    p.op("dve", lambda e, xb_=xb_, cc=cc, w=w, j=j: e.scalar_tensor_tensor(
                        out=u[:, :w], in0=xb_[:, j:j + w], scalar=convw[:, cc, j:j + 1], in1=u[:, :w], op0=ALU.mult, op1=ALU.add),
                        reads=R(xb_, convw, u), writes=R(u))
                p.op("pool", lambda e, w=w: e.tensor_copy(out=ub[:, :w], in_=u[:, :w]), reads=R(u), writes=R(ub))
                p.op("pe", lambda e, cc=cc, w=w: e.matmul(bk[5][:, :w], wab[:, cc, 0:128], ub[:, :w], start=True, stop=True),
                     reads=R(wab, ub), writes=R(bk[5]))
                p.op("pe", lambda e, cc=cc, w=w: e.matmul(bk[6][:, :w], wab[:, cc, 128:256], ub[:, :w], start=True, stop=True),
                     reads=R(wab, ub), writes=R(bk[6]))
                p.op("act", lambda e, cc=cc, w=w: e.activation(out=r_[:, :w], in_=bk[5][:, :w], func=AF.Sigmoid, bias=lvec[:, cc, 1:2]),
                     reads=R(bk[5], lvec), writes=R(r_))
                p.op("act", lambda e, cc=cc, w=w: e.activation(out=gi[:, :w], in_=bk[6][:, :w], func=AF.Sigmoid, bias=lvec[:, cc, 2:3]),
                     reads=R(bk[6], lvec), writes=R(gi))
                p.op("pool", lambda e, w=w: e.tensor_tensor(out=gx[:, :w], in0=xg[:, :w], in1=xg[:, :w], op=ALU.mult), reads=R(xg), writes=R(gx))
                p.op("pool", lambda e, w=w: e.tensor_scalar(out=gx[:, :w], in0=gx[:, :w], scalar1=0.044715, scalar2=1.0, op0=ALU.mult, op1=ALU.add),
                     reads=R(gx), writes=R(gx))
                p.op("pool", lambda e, w=w: e.tensor_tensor(out=gx[:, :w], in0=gx[:, :w], in1=xg[:, :w], op=ALU.mult), reads=R(gx, xg), writes=R(gx))
                p.op("act", lambda e, w=w: e.activation(out=sg[:, :w], in_=gx[:, :w], func=AF.Sigmoid, scale=GELU_K), reads=R(gx), writes=R(sg))
                p.op("act", lambda e, cc=cc, w=w: e.activation(out=a_[:, :w], in_=r_[:, :w], func=AF.Exp, scale=sneg[:, cc:cc + 1]),
                     reads=R(r_, sneg), writes=R(a_))
                p.op("act", lambda e, cc=cc, w=w: e.activation(out=a2[:, :w], in_=r_[:, :w], func=AF.Exp, scale=sneg2[:, cc:cc + 1]),
                     reads=R(r_, sneg2), writes=R(a2))
                p.op("act", lambda e, w=w: e.activation(out=ml[:, :w], in_=a2[:, :w], func=AF.Ln, scale=-1.0, bias=1.0), reads=R(a2), writes=R(ml))
                p.op("act", lambda e, w=w: e.activation(out=ml[:, :w], in_=ml[:, :w], func=AF.Exp, scale=0.5), reads=R(ml), writes=R(ml))
                p.op("pool", lambda e, w=w: e.tensor_tensor(out=inp[:, :w], in0=gi[:, :w], in1=u[:, :w], op=ALU.mult), reads=R(gi, u), writes=R(inp))
                p.op("dve", lambda e, w=w: e.tensor_tensor(out=inp[:, :w], in0=inp[:, :w], in1=ml[:, :w], op=ALU.mult), reads=R(inp, ml), writes=R(inp))
                hb = hbuf[cc][i % 2]
                if i == 0:
                    p.op("dve", lambda e, hb=hb, w=w: e.tensor_tensor_scan(hb[:, :w], a_[:, :w], inp[:, :w], 0.0, ALU.mult, ALU.add),
                         reads=R(a_, inp), writes=R(hb))
                else:
                    hp = hbuf[cc][(i - 1) % 2]
                    p.op("dve", lambda e, hb=hb, hp=hp, w=w, wprev=wprev: e.tensor_tensor_scan(
                        hb[:, :w], a_[:, :w], inp[:, :w], hp[:, wprev - 1:wprev], ALU.mult, ALU.add),
                        reads=R(a_, inp, hp), writes=R(hb))
                p.op("pool", lambda e, w=w: e.tensor_tensor(out=sg[:, :w], in0=sg[:, :w], in1=xg[:, :w], op=ALU.mult), reads=R(sg, xg), writes=R(sg))
                lo_ = lost[cc]
                p.op("dve", lambda e, hb=hb, lo_=lo_, w=w: e.tensor_tensor(out=lo_[:, :w], in0=hb[:, :w], in1=sg[:, :w], op=ALU.mult),
                     reads=R(hb, sg), writes=R(lo_))
                p.dma("sp", lambda e, lo_=lo_, cc=cc, t0=t0, w=w: e.dma_start(
                    out=attT[rows[2] + cc * 128:rows[2] + (cc + 1) * 128, t0:t0 + w], in_=lo_[:, :w]), reads=R(lo_), writes=[(attT_t, ("l", cc, t0))])
            wprev = w
        cx.reset(mark1)
    (emit_attention2 if ATTN_INTERLEAVE else emit_attention)(cx, Lp, tiles, QK, Vd, CB, attT, attT_t, rows, ones_bf, negu, mle, mlt)


def load_consts(cx, mle_d, mlt_d, negu_d, Lp):
    p = cx.p
    NB = Lp // 128
    S = {}
    S["QK"] = cx.dram("QKs", [8, 128, Lp], BF16)
    S["Vd"] = cx.dram("Vds", [4, 128, NB, 128], BF16)
    S["CB"] = cx.dram("CBs", [2, 3, Lp], BF16)
    ones_bf = cx.sb("ones_bf", [128, 128], BF16)
    p.op("pool", lambda e: e.memset(ones_bf[:], 1.0), writes=R(ones_bf))
    negu = cx.sb("negu", [128, 128], BF16)
    p.dma("pool", lambda e: e.dma_start(out=negu[:], in_=negu_d), writes=R(negu))
    mle = cx.sb("mle", [128, 4, 512], BF16)
    mlt = cx.sb("mlt", [128, 4, 512], BF16)
    p.dma("pool", lambda e: e.dma_start(out=mle[:], in_=mle_d), writes=R(mle))
    p.dma("pool", lambda e: e.dma_start(out=mlt[:], in_=mlt_d), writes=R(mlt))
    S.update(ones_bf=ones_bf, negu=negu, mle=mle, mlt=mlt)
    S["mark0"] = cx.mark()
    return S


A_IN = [("gmix", [128, 8]), ("w_qk", [D, 1024]), ("w_v", [D, 512]), ("w_f", [D, 2]), ("w_lru", [D, 512]), ("nb_f", [2, 1]),
        ("conv_w", [128, 2, 4]), ("lvec", [128, 2, 4]), ("wab", [128, 2, 256])]


def build_A(Lp):
    from contextlib import ExitStack
    nc = bass.Bass("TRN2", target_bir_lowering=False)

    def din(name, shape, dt=F32):
        return nc.dram_tensor(name, shape, dt, kind="ExternalInput").ap()

    hT = din("hT", [D, Lp])
    I = {k: din(k, shp) for k, shp in A_IN}
    mle_d = din("mask_le", [128, 4, 512])
    mlt_d = din("mask_lt", [128, 4, 512])
    negu_d = din("negU", [128, 128])
    attT = nc.dram_tensor("attT", [768, Lp], BF16, kind="ExternalOutput").ap()
    with ExitStack() as st:
        cx = Ctx(nc, st)
        S = load_consts(cx, mle_d, mlt_d, negu_d, Lp)
        stage_A(cx, Lp, I, hT, TT(hT, "hT"), attT, TT(attT, "attT"), (0, 256, 512), S)
        cx.p.final_wait("sp")
        cx.p.build(st)
    return nc


def emit_attention(cx, Lp, tiles, QK, Vd, CB, attT, attT_t, rows, ones_bf, negu, mle, mlt):
    p = cx.p
    bk = cx.banks
    NB = Lp // 128
    KT = [cx.sb(f"KT{i}", [128, Lp], BF16) for i in range(2)]
    VV = [cx.sb(f"VV{i}", [128, NB, 128], BF16) for i in range(2)]
    ck6 = cx.sb("ck6", [6, Lp], BF16)
    cq6 = cx.sb("cq6", [6, Lp], BF16)
    qT = [cx.sb(f"qT{i}", [128, 512], BF16) for i in range(2)]
    pT = [cx.sb(f"pT{i}", [128, 512], BF16) for i in range(3)]
    rd = cx.sb("rd", [128, 512])
    osb = [cx.sb(f"osb{i}", [128, 512], BF16) for i in range(2)]
    esb = [cx.sb(f"esb{i}", [128, 512]) for i in range(2)]
    spb = [cx.sb(f"spb{i}", [128, 512], BF16) for i in range(3)]
    Tsb = [cx.sb(f"Tsb{i}", [128, 512]) for i in range(2)]
    arg = [cx.sb(f"arg{i}", [128, 512]) for i in range(2)]
    p.op("pool", lambda e: e.memset(ck6[:], 1.0), writes=R(ck6))
    p.op("pool", lambda e: e.memset(cq6[:], -1.0), writes=R(cq6))
    QKa = QK.h.ap()
    Vda = Vd.h.ap()
    CBa = CB.h.ap()
    qc = 0
    heads = [("fox", 0), ("fox", 1), ("sb", 0), ("sb", 1)]
    def do_head(hi, kind, h, qc):
        if kind == "fox":
            qch, kch, vi, row0 = h, 2 + h, h, rows[0] + h * 128
        else:
            qch, kch, vi, row0 = 4 + h, 6 + h, 2 + h, rows[1] + h * 128
        kt = KT[hi % 2]
        vv = VV[hi % 2]
        p.dma("sp", lambda e, kt=kt, kch=kch: e.dma_start(out=kt[:, :], in_=QKa[kch]), reads=R(QK), writes=R(kt))
        p.dma("sp", lambda e, vv=vv, vi=vi: e.dma_start(out=vv[:, :, :], in_=Vda[vi]), reads=R(Vd), writes=R(vv))
        if kind == "fox":
            p.dma("sp", lambda e, h=h: e.dma_start(out=ck6[3:6, :], in_=CBa[h]), reads=R(CB), writes=R(ck6))
            p.dma("sp", lambda e, h=h: e.dma_start(out=cq6[0:3, :], in_=CBa[h]), reads=R(CB), writes=R(cq6))
        units = []
        for j, (t0, w) in enumerate(tiles):
            kb_max = (t0 + w - 1) // 128
            kbs = list(range(kb_max + 1)) if kind == "fox" else list(range(kb_max, -1, -1))
            for n_, kb in enumerate(kbs):
                units.append(dict(j=j, t0=t0, w=w, kb=kb, first=(n_ == 0), last=(n_ == len(kbs) - 1), qi=qc + j,
                                  diag=(kb * 128 + 127 > t0), oi=max(0, (kb * 128 - t0) // 128)))
        NU = len(units)
        qloaded = set()

        def load_q(u):
            if u["qi"] in qloaded:
                return
            qloaded.add(u["qi"])
            q_ = qT[u["qi"] % 2]
            t0, w = u["t0"], u["w"]
            p.dma("sp", lambda e, q_=q_, t0=t0, w=w: e.dma_start(out=q_[:, :w], in_=QKa[qch, :, t0:t0 + w]), reads=R(QK), writes=R(q_))

        def store_o(u, ob):
            t0, w = u["t0"], u["w"]
            p.dma("sp", lambda e, ob=ob, t0=t0, w=w: e.dma_start(out=attT[row0:row0 + 128, t0:t0 + w], in_=ob[:, :w]),
                  reads=R(ob), writes=[(attT_t, (row0, t0))])

        if kind == "fox":
            def f1(i):
                u = units[i]
                load_q(u)
                if i + 1 < NU:
                    load_q(units[i + 1])
                q_ = qT[u["qi"] % 2]
                S = bk[i % 2]
                kb, t0, w = u["kb"], u["t0"], u["w"]
                p.op("pe", lambda e: e.matmul(S[:, :w], kt[:, kb * 128:(kb + 1) * 128], q_[:, :w], start=True, stop=False),
                     reads=R(kt, q_), writes=R(S))
                p.op("pe", lambda e: e.matmul(S[:, :w], ck6[:, kb * 128:(kb + 1) * 128], cq6[:, t0:t0 + w], start=False, stop=True),
                     reads=R(ck6, cq6), writes=R(S))

            def f2(i):
                u = units[i]
                S = bk[i % 2]
                P_ = pT[i % 3]
                w, oi = u["w"], u["oi"]
                p.op("act", lambda e: e.activation(out=P_[:, :w], in_=S[:, :w], func=AF.Exp), reads=R(S), writes=R(P_))
                if u["diag"]:
                    p.op("pool", lambda e: e.tensor_tensor(out=P_[:, :w], in0=P_[:, :w], in1=mle[:, oi, :w], op=ALU.mult),
                         reads=R(P_, mle), writes=R(P_))

            def f3(i):
                u = units[i]
                P_ = pT[i % 3]
                O = bk[2 + u["qi"] % 2]
                Dn = bk[4 + u["qi"] % 2]
                kb, w, first, last = u["kb"], u["w"], u["first"], u["last"]
                p.op("pe", lambda e: e.matmul(O[:, :w], vv[:, kb, :], P_[:, :w], start=first, stop=last), reads=R(vv, P_), writes=R(O))
                p.op("pe", lambda e: e.matmul(Dn[:, :w], ones_bf[:, :], P_[:, :w], start=first, stop=last), reads=R(ones_bf, P_), writes=R(Dn))
                if last:
                    ob = osb[u["qi"] % 2]
                    p.op("dve", lambda e: e.reciprocal(out=rd[:, :w], in_=Dn[:, :w]), reads=R(Dn), writes=R(rd))
                    p.op("dve", lambda e: e.tensor_tensor(out=ob[:, :w], in0=O[:, :w], in1=rd[:, :w], op=ALU.mult), reads=R(O, rd), writes=R(ob))
                    store_o(u, ob)

            for i in range(NU + 1):
                if i < NU:
                    f1(i)
                    f2(i)
                if i >= 1:
                    f3(i - 1)
        else:
            zb = (bk[0], bk[1], bk[6])
            tstate = {}

            def g1(i):
                u = units[i]
                load_q(u)
                if i + 1 < NU:
                    load_q(units[i + 1])
                q_ = qT[u["qi"] % 2]
                Z = zb[i % 3]
                kb, w = u["kb"], u["w"]
                p.op("pe", lambda e: e.matmul(Z[:, :w], kt[:, kb * 128:(kb + 1) * 128], q_[:, :w], start=True, stop=True), reads=R(kt, q_), writes=R(Z))

            def g2(i):
                u = units[i]
                Z = zb[i % 3]
                e_ = esb[i % 2]
                s_ = spb[i % 3]
                w, oi = u["w"], u["oi"]
                p.op("act", lambda e: e.activation(out=e_[:, :w], in_=Z[:, :w], func=AF.Exp), reads=R(Z), writes=R(e_))
                p.op("act", lambda e: e.activation(out=s_[:, :w], in_=e_[:, :w], func=AF.Ln, bias=1.0), reads=R(e_), writes=R(s_))
                if u["diag"]:
                    p.op("pool", lambda e: e.tensor_tensor(out=s_[:, :w], in0=s_[:, :w], in1=mlt[:, oi, :w], op=ALU.mult), reads=R(s_, mlt), writes=R(s_))

            def g3(i):
                u = units[i]
                Z = zb[i % 3]
                C = bk[4 + i % 2]
                s_ = spb[i % 3]
                w = u["w"]
                p.op("pe", lambda e: e.matmul(Z[:, :w], negu[:, :], s_[:, :w], start=False, stop=True), reads=R(negu, s_), writes=R(Z))
                if not u["last"]:
                    p.op("pe", lambda e: e.matmul(C[:, :w], ones_bf[:, :], s_[:, :w], start=True, stop=True), reads=R(ones_bf, s_), writes=R(C))

            def g4(i):
                u = units[i]
                Z = zb[i % 3]
                C = bk[4 + i % 2]
                w = u["w"]
                if u["first"]:
                    tstate["n"] = 0
                    tstate["cur"] = None
                tcur = tstate["cur"]
                if not u["first"]:
                    ar = arg[i % 2]
                    p.op("dve", lambda e: e.tensor_tensor(out=ar[:, :w], in0=Z[:, :w], in1=tcur[:, :w], op=ALU.subtract), reads=R(Z, tcur), writes=R(ar))
                if not u["last"]:
                    tnext = Tsb[tstate["n"] % 2]
                    tstate["n"] += 1
                    if u["first"]:
                        p.op("dve", lambda e: e.tensor_copy(out=tnext[:, :w], in_=C[:, :w]), reads=R(C), writes=R(tnext))
                    else:
                        p.op("dve", lambda e: e.tensor_tensor(out=tnext[:, :w], in0=C[:, :w], in1=tcur[:, :w], op=ALU.add), reads=R(C, tcur), writes=R(tnext))
                    tstate["cur"] = tnext

            def g5(i):
                u = units[i]
                Z = zb[i % 3]
                A_ = pT[i % 3]
                w, oi = u["w"], u["oi"]
                src_ = Z if u["first"] else arg[i % 2]
                p.op("act", lambda e: e.activation(out=A_[:, :w], in_=src_[:, :w], func=AF.Exp), reads=R(src_), writes=R(A_))
                if u["diag"]:
                    p.op("pool", lambda e: e.tensor_tensor(out=A_[:, :w], in0=A_[:, :w], in1=mlt[:, oi, :w], op=ALU.mult), reads=R(A_, mlt), writes=R(A_))

            def g6(i):
                u = units[i]
                A_ = pT[i % 3]
                O = bk[2 + u["qi"] % 2]
                kb, w, first, last = u["kb"], u["w"], u["first"], u["last"]
                p.op("pe", lambda e: e.matmul(O[:, :w], vv[:, kb, :], A_[:, :w], start=first, stop=last), reads=R(vv, A_), writes=R(O))
                if last:
                    ob = osb[u["qi"] % 2]
                    p.op("act", lambda e: e.activation(out=ob[:, :w], in_=O[:, :w], func=AF.Copy), reads=R(O), writes=R(ob))
                    store_o(u, ob)

            for i in range(NU + 2):
                if i < NU:
                    g1(i)
                    g2(i)
                if 1 <= i <= NU:
                    g3(i - 1)
                    g4(i - 1)
                    g5(i - 1)
                if i >= 2:
                    g6(i - 2)

    for hi, (kind, h) in enumerate(heads):
        do_head(hi, kind, h, qc)
        qc += len(tiles)


def emit_attention2(cx, Lp, tiles, QK, Vd, CB, attT, attT_t, rows, ones_bf, negu, mle, mlt):
    p = cx.p
    bk = cx.banks
    NB = Lp // 128
    QKa, Vda, CBa = QK.h.ap(), Vd.h.ap(), CB.h.ap()
    KTf = cx.sb("KTf", [128, Lp], BF16)
    KTs = cx.sb("KTs", [128, Lp], BF16)
    VVf = cx.sb("VVf", [128, NB, 128], BF16)
    VVs = cx.sb("VVs", [128, NB, 128], BF16)
    ck6 = cx.sb("ck6", [6, Lp], BF16)
    cq6 = cx.sb("cq6", [6, Lp], BF16)
    qTf = [cx.sb(f"qTf{i}", [128, 512], BF16) for i in range(2)]
    qTs = [cx.sb(f"qTs{i}", [128, 512], BF16) for i in range(2)]
    pTf = [cx.sb(f"pTf{i}", [128, 512], BF16) for i in range(3)]
    pTs = [cx.sb(f"pTs{i}", [128, 512], BF16) for i in range(3)]
    rd = cx.sb("rd", [128, 512])
    osbf = [cx.sb(f"osbf{i}", [128, 512], BF16) for i in range(2)]
    osbs = [cx.sb(f"osbs{i}", [128, 512], BF16) for i in range(2)]
    esb = [cx.sb(f"esb{i}", [128, 512]) for i in range(2)]
    spb = [cx.sb(f"spb{i}", [128, 512], BF16) for i in range(3)]
    Tsb = [cx.sb(f"Tsb{i}", [128, 512]) for i in range(2)]
    arg = [cx.sb(f"arg{i}", [128, 512]) for i in range(2)]
    p.op("pool", lambda e: e.memset(ck6[:], 1.0), writes=R(ck6))
    p.op("pool", lambda e: e.memset(cq6[:], -1.0), writes=R(cq6))
    mneg = cx.sb("mneg", [128, 4, 512])
    sdg = [cx.sb(f"sdg{i}", [128, 512]) for i in range(2)]
    p.op("dve", lambda e: e.tensor_scalar(out=mneg[:, :, :], in0=mle[:, :, :], scalar1=30000.0, scalar2=-30000.0, op0=ALU.mult, op1=ALU.add),
         reads=R(mle), writes=R(mneg))
    Sb = (bk[0], bk[1])
    Of, Df = bk[2], bk[3]
    Zb = (bk[4], bk[5])
    Cb, Os = bk[6], bk[7]

    def mk_units(order_desc):
        units = []
        for j, (t0, w) in enumerate(tiles):
            kb_max = (t0 + w - 1) // 128
            kbs = list(range(kb_max, -1, -1)) if order_desc else list(range(kb_max + 1))
            for n_, kb in enumerate(kbs):
                units.append(dict(j=j, t0=t0, w=w, kb=kb, first=(n_ == 0), last=(n_ == len(kbs) - 1),
                                  diag=(kb * 128 + 127 > t0), oi=max(0, (kb * 128 - t0) // 128)))
        return units

    def do_pair(h):
        UF = mk_units(False)
        US = mk_units(True)
        NU = len(UF)
        assert len(US) == NU
        p.dma("sp", lambda e: e.dma_start(out=KTf[:, :], in_=QKa[2 + h]), reads=R(QK), writes=R(KTf))
        p.dma("sp", lambda e: e.dma_start(out=VVf[:, :, :], in_=Vda[h]), reads=R(Vd), writes=R(VVf))
        p.dma("sp", lambda e: e.dma_start(out=ck6[3:6, :], in_=CBa[h]), reads=R(CB), writes=R(ck6))
        p.dma("sp", lambda e: e.dma_start(out=cq6[0:3, :], in_=CBa[h]), reads=R(CB), writes=R(cq6))
        p.dma("sp", lambda e: e.dma_start(out=KTs[:, :], in_=QKa[6 + h]), reads=R(QK), writes=R(KTs))
        p.dma("sp", lambda e: e.dma_start(out=VVs[:, :, :], in_=Vda[2 + h]), reads=R(Vd), writes=R(VVs))
        rowf = rows[0] + h * 128
        rows_ = rows[1] + h * 128
        loaded = {"f": set(), "s": set()}

        def load_q(kind, u):
            j = u["j"]
            if j in loaded[kind]:
                return
            loaded[kind].add(j)
            q_ = (qTf if kind == "f" else qTs)[j % 2]
            qch = h if kind == "f" else 4 + h
            t0, w = u["t0"], u["w"]
            p.dma("sp", lambda e: e.dma_start(out=q_[:, :w], in_=QKa[qch, :, t0:t0 + w]), reads=R(QK), writes=R(q_))

        def store_o(u, ob, row0):
            t0, w = u["t0"], u["w"]
            p.dma("sp", lambda e: e.dma_start(out=attT[row0:row0 + 128, t0:t0 + w], in_=ob[:, :w]), reads=R(ob), writes=[(attT_t, (row0, t0))])

        def f1(i):
            u = UF[i]
            load_q("f", u)
            if i + 1 < NU:
                load_q("f", UF[i + 1])
            q_ = qTf[u["j"] % 2]
            S = Sb[i % 2]
            kb, t0, w = u["kb"], u["t0"], u["w"]
            p.op("pe", lambda e: e.matmul(S[:, :w], KTf[:, kb * 128:(kb + 1) * 128], q_[:, :w], start=True, stop=False), reads=R(KTf, q_), writes=R(S))
            p.op("pe", lambda e: e.matmul(S[:, :w], ck6[:, kb * 128:(kb + 1) * 128], cq6[:, t0:t0 + w], start=False, stop=True), reads=R(ck6, cq6), writes=R(S))

        def f2(i):
            u = UF[i]
            S = Sb[i % 2]
            P_ = pTf[i % 3]
            w, oi = u["w"], u["oi"]
            if u["diag"]:
                sd = sdg[i % 2]
                p.op("dve", lambda e: e.tensor_tensor(out=sd[:, :w], in0=S[:, :w], in1=mneg[:, oi, :w], op=ALU.add), reads=R(S, mneg), writes=R(sd))
                p.op("act", lambda e: e.activation(out=P_[:, :w], in_=sd[:, :w], func=AF.Exp), reads=R(sd), writes=R(P_))
            else:
                p.op("act", lambda e: e.activation(out=P_[:, :w], in_=S[:, :w], func=AF.Exp), reads=R(S), writes=R(P_))

        def f3(i):
            u = UF[i]
            P_ = pTf[i % 3]
            kb, w, first, last = u["kb"], u["w"], u["first"], u["last"]
            p.op("pe", lambda e: e.matmul(Of[:, :w], VVf[:, kb, :], P_[:, :w], start=first, stop=last), reads=R(VVf, P_), writes=R(Of))
            p.op("pe", lambda e: e.matmul(Df[:, :w], ones_bf[:, :], P_[:, :w], start=first, stop=last), reads=R(ones_bf, P_), writes=R(Df))
            if last:
                ob = osbf[u["j"] % 2]
                p.op("dve", lambda e: e.reciprocal(out=rd[:, :w], in_=Df[:, :w]), reads=R(Df), writes=R(rd))
                p.op("dve", lambda e: e.tensor_tensor(out=ob[:, :w], in0=Of[:, :w], in1=rd[:, :w], op=ALU.mult), reads=R(Of, rd), writes=R(ob))
                store_o(u, ob, rowf)

        tstate = {}

        def g1(i):
            u = US[i]
            load_q("s", u)
            if i + 1 < NU:
                load_q("s", US[i + 1])
            q_ = qTs[u["j"] % 2]
            Z = Zb[i % 2]
            kb, w = u["kb"], u["w"]
            p.op("pe", lambda e: e.matmul(Z[:, :w], KTs[:, kb * 128:(kb + 1) * 128], q_[:, :w], start=True, stop=True), reads=R(KTs, q_), writes=R(Z))

        def g2(i):
            u = US[i]
            Z = Zb[i % 2]
            e_ = esb[i % 2]
            s_ = spb[i % 3]
            w, oi = u["w"], u["oi"]
            p.op("act", lambda e: e.activation(out=e_[:, :w], in_=Z[:, :w], func=AF.Exp), reads=R(Z), writes=R(e_))
            p.op("act", lambda e: e.activation(out=s_[:, :w], in_=e_[:, :w], func=AF.Ln, bias=1.0), reads=R(e_), writes=R(s_))
            if u["diag"]:
                p.op("pool", lambda e: e.tensor_tensor(out=s_[:, :w], in0=s_[:, :w], in1=mlt[:, oi, :w], op=ALU.mult), reads=R(s_, mlt), writes=R(s_))

        def g3(i):
            u = US[i]
            Z = Zb[i % 2]
            s_ = spb[i % 3]
            w = u["w"]
            p.op("pe", lambda e: e.matmul(Z[:, :w], negu[:, :], s_[:, :w], start=False, stop=True), reads=R(negu, s_), writes=R(Z))
            if not u["last"]:
                p.op("pe", lambda e: e.matmul(Cb[:, :w], ones_bf[:, :], s_[:, :w], start=True, stop=True), reads=R(ones_bf, s_), writes=R(Cb))

        def g4(i):
            u = US[i]
            Z = Zb[i % 2]
            w = u["w"]
            if u["first"]:
                tstate["n"] = 0
                tstate["cur"] = None
            tcur = tstate["cur"]
            if not u["first"]:
                ar = arg[i % 2]
                p.op("dve", lambda e: e.tensor_tensor(out=ar[:, :w], in0=Z[:, :w], in1=tcur[:, :w], op=ALU.subtract), reads=R(Z, tcur), writes=R(ar))
            if not u["last"]:
                tnext = Tsb[tstate["n"] % 2]
                tstate["n"] += 1
                if u["first"]:
                    p.op("dve", lambda e: e.tensor_copy(out=tnext[:, :w], in_=Cb[:, :w]), reads=R(Cb), writes=R(tnext))
                else:
                    p.op("dve", lambda e: e.tensor_tensor(out=tnext[:, :w], in0=Cb[:, :w], in1=tcur[:, :w], op=ALU.add), reads=R(Cb, tcur), writes=R(tnext))
                tstate["cur"] = tnext

        def g5(i):
            u = US[i]
            Z = Zb[i % 2]
            A_ = pTs[i % 3]
            w, oi = u["w"], u["oi"]
            src_ = Z if u["first"] else arg[i % 2]
            p.op("act", lambda e: e.activation(out=A_[:, :w], in_=src_[:, :w], func=AF.Exp), reads=R(src_), writes=R(A_))
            if u["diag"]:
                p.op("pool", lambda e: e.tensor_tensor(out=A_[:, :w], in0=A_[:, :w], in1=mlt[:, oi, :w], op=ALU.mult), reads=R(A_, mlt), writes=R(A_))

        def g6(i):
            u = US[i]
            A_ = pTs[i % 3]
            kb, w, first, last = u["kb"], u["w"], u["first"], u["last"]
            p.op("pe", lambda e: e.matmul(Os[:, :w], VVs[:, kb, :], A_[:, :w], start=first, stop=last), reads=R(VVs, A_), writes=R(Os))
            if last:
                ob = osbs[u["j"] % 2]
                p.op("act", lambda e: e.activation(out=ob[:, :w], in_=Os[:, :w], func=AF.Copy), reads=R(Os), writes=R(ob))
                store_o(u, ob, rows_)

        for i in range(NU + 2):
            if i < NU:
                f1(i)
                g1(i)
                f2(i)
                g2(i)
            if 1 <= i <= NU:
                g3(i - 1)
                g4(i - 1)
                g5(i - 1)
                f3(i - 1)
            if i >= 2:
                g6(i - 2)

    for h in range(2):
        do_pair(h)


def _consts():
    k = np.arange(128)[:, None, None]
    o = np.arange(4)[None, :, None] * 128
    q = np.arange(512)[None, None, :]
    mle = ((k + o) <= q).astype(np.float32)
    mlt = ((k + o) < q).astype(np.float32)
    j = np.arange(128)[:, None]
    s = np.arange(128)[None, :]
    negu = np.where(j >= s, -1.0, 0.0).astype(np.float32)
    return mle, mlt, negu


def _pc(v):
    return np.ascontiguousarray(v.reshape(-1, 128).T)


def prep_A(layer, half, hT_b, P):
    w_in = P["w_in"][layer]
    hh = [2 * half, 2 * half + 1]

    def cols(base, width=128):
        return np.concatenate([w_in[:, base + h * width: base + (h + 1) * width] for h in hh], axis=1)

    w_qk = np.concatenate([cols(0), cols(512), cols(1540), cols(2052)], axis=1)
    w_v = np.concatenate([cols(1024), cols(2564)], axis=1)
    w_f = np.concatenate([w_in[:, 1536 + h:1537 + h] for h in hh], axis=1)
    nb_f = np.ascontiguousarray(-P["b_forget"][layer][hh].reshape(2, 1))
    c0 = half * 256
    w_lru = np.concatenate([w_in[:, 3076 + c0:3076 + c0 + 256], w_in[:, 3588 + c0:3588 + c0 + 256]], axis=1)
    convw = np.zeros((128, 2, 4), np.float32)
    lvec = np.zeros((128, 2, 4), np.float32)
    wab = np.zeros((128, 2, 256), np.float32)
    for cc in range(2):
        ch = c0 + cc * 128 + np.arange(128)
        convw[:, cc, :] = P["conv_w"][layer][:, ch].T
        lvec[:, cc, 0] = P["conv_b"][layer][ch]
        lvec[:, cc, 1] = P["lru_ba"][layer][ch]
        lvec[:, cc, 2] = P["lru_bx"][layer][ch]
        lvec[:, cc, 3] = P["lru_lambda"][layer][ch]
        for nl in range(2):
            n = (c0 + cc * 128) // 64 + nl
            wab[nl * 64:(nl + 1) * 64, cc, nl * 64:(nl + 1) * 64] = P["lru_wa"][layer][n]
            wab[nl * 64:(nl + 1) * 64, cc, 128 + nl * 64:128 + (nl + 1) * 64] = P["lru_wx"][layer][n]
    mle, mlt, negu = _consts()
    f = np.ascontiguousarray
    return {
        "hT": (None if hT_b is None else f(hT_b)), "gmix": _pc(P["g_mix"][layer]), "w_qk": f(w_qk), "w_v": f(w_v), "w_f": f(w_f),
        "w_lru": f(w_lru), "nb_f": nb_f, "conv_w": convw, "lvec": lvec, "wab": wab,
        "mask_le": mle, "mask_lt": mlt, "negU": negu,
    }


B_IN_COMMON = [("gmix", [128, 8]), ("gffn", [128, 8]), ("w_g", [D, 3072]), ("b_g", [128, 24]), ("w_o3", [1536, D]), ("w_out", [D, D])]
B_IN_DENSE = [("ffn_wg", [D, 2816]), ("ffn_wu", [D, 2816]), ("ffn_wd", [2816, D])]


def b_in_moe(n_exp, dff_e):
    return [("router_w", [128, 8, 8]), ("moe_wg", [n_exp, D, dff_e]), ("moe_wu", [n_exp, D, dff_e]), ("moe_wd", [n_exp, dff_e, D]),
            ("gfin", [128, 8]), ("ident", [128, 128]), ("sel8", [8, 8, 128])]


def stage_B(cx, Tn, kind, I, hT, hT_t, attT, attT_t, hout, hout_t, S, tag="", n_exp=8, dff_e=3584):
    p = cx.p
    bk = cx.banks
    tiles = [(t0, min(512, Tn - t0)) for t0 in range(0, Tn, 512)]
    gmix_d, gffn_d, wg_d, bg_d, wo3_d, wout_d = I["gmix"], I["gffn"], I["w_g"], I["b_g"], I["w_o3"], I["w_out"]
    if kind == "dense":
        fg_d, fu_d, fd_d = I["ffn_wg"], I["ffn_wu"], I["ffn_wd"]
    else:
        rw_d, mg_d, mu_d, md_d = I["router_w"], I["moe_wg"], I["moe_wu"], I["moe_wd"]
        gfin_d, ident_d, sel_d = I["gfin"], I["ident"], I["sel8"]
    H1 = cx.dram("H1s" + tag, [D, Tn], F32)
    HN = cx.dram("HNs" + tag, [D, Tn], BF16)
    H1a = H1.h.ap()
    HNa = HN.h.ap()

    def fm(ap_):
        return ap_.rearrange("(c p) t -> p c t", p=128)

    ones_bf = S["ones_bf"]
    cx.reset(S["mark0"])
    gmix = cx.sb("gmix_sb", [128, 8])
    gffn = cx.sb("gffn_sb", [128, 8])
    p.dma("sp", lambda e: e.dma_start(out=gmix[:], in_=gmix_d), writes=R(gmix))
    p.dma("sp", lambda e: e.dma_start(out=gffn[:], in_=gffn_d), writes=R(gffn))
    lnv = cx.sb("lnv", [128, 512])
    rstd = cx.sb("rstd", [128, 512])
    sq = cx.sb("sq", [128, 8, 512], BF16)
    mark0 = cx.mark()

    wg = cx.sb("wg", [128, 8, 3072], BF16)
    wo3 = cx.sb("wo3", [128, 12, 1024], BF16)
    wout = cx.sb("wout", [128, 8, 1024], BF16)
    bg = cx.sb("bg", [128, 24])
    p.dma("sp", lambda e: e.dma_start(out=bg[:], in_=bg_d), writes=R(bg))
    for c in range(KC):
        p.dma("pool", lambda e, c=c: e.dma_start(out=wg[:, c, :], in_=wg_d[c * 128:(c + 1) * 128, :]), writes=R(wg))
    for c in range(12):
        p.dma("pool", lambda e, c=c: e.dma_start(out=wo3[:, c, :], in_=wo3_d[c * 128:(c + 1) * 128, :]), writes=R(wo3))
    for c in range(KC):
        p.dma("pool", lambda e, c=c: e.dma_start(out=wout[:, c, :], in_=wout_d[c * 128:(c + 1) * 128, :]), writes=R(wout))
    htb = [cx.sb(f"ht{i}", [128, 8, 512]) for i in range(2)]
    atb = [cx.sb(f"at{i}", [128, 12, 512], BF16) for i in range(2)]
    xn = cx.sb("xn", [128, 8, 512], BF16)
    gsb = [cx.sb(f"gs{i}", [128, 3, 512], BF16) for i in range(2)]
    tq = [[cx.sb(f"tq{i}_{j}", [128, 512]) for j in range(3)] for i in range(1)]
    mrg = cx.sb("mrg", [128, 8, 512], BF16)
    hnb = [cx.sb(f"hn_{i}", [128, 8, 512], BF16) for i in range(1)]
    rot = [bk[1], bk[2], bk[3], bk[4], bk[5], bk[6], bk[7]]
    rc = [0]

    def nextbank():
        b = rot[rc[0] % len(rot)]
        rc[0] += 1
        return b

    for i, (t0, w) in enumerate(tiles):
        ht = htb[i % 2]
        at = atb[i % 2]
        p.dma("sp", lambda e, ht=ht, t0=t0, w=w: e.dma_start(out=ht[:, :, :w], in_=fm(hT)[:, :, t0:t0 + w]), reads=R(hT_t), writes=R(ht))
        p.dma("sp", lambda e, at=at, t0=t0, w=w: e.dma_start(out=at[:, :, :w], in_=fm(attT)[:, :, t0:t0 + w]), reads=R(attT_t), writes=R(at))
        emit_rmsnorm(cx, ht, w, gmix, ones_bf, sq, bk[0], lnv, rstd, xn)
        for m in range(8):
            gs = gsb[m % 2]
            tqm = tq[0]
            for br in range(3):
                b = nextbank()
                emit_proj(cx, b, wg, br * 1024 + m * 128, 128, xn, w)
                p.op("act", lambda e, gs=gs, br=br, b=b, m=m, w=w: e.activation(
                    out=gs[:, br, :w], in_=b[:, :w], func=AF.Sigmoid, bias=bg[:, br * 8 + m:br * 8 + m + 1]),
                    reads=R(b, bg), writes=R(gs))
            for br in range(3):
                b = nextbank()
                for kc in range(4):
                    p.op("pe", lambda e, b=b, br=br, kc=kc, m=m, at=at, w=w: e.matmul(
                        b[:, :w], wo3[:, br * 4 + kc, m * 128:(m + 1) * 128], at[:, br * 4 + kc, :w],
                        start=(kc == 0), stop=(kc == 3)), reads=R(wo3, at), writes=R(b))
                p.op("dve", lambda e, b=b, br=br, gs=gs, tqm=tqm, w=w: e.tensor_tensor(
                    out=tqm[br][:, :w], in0=b[:, :w], in1=gs[:, br, :w], op=ALU.mult), reads=R(b, gs), writes=R(tqm[br]))
            p.op("pool", lambda e, tqm=tqm, w=w: e.tensor_tensor(out=tqm[0][:, :w], in0=tqm[0][:, :w], in1=tqm[1][:, :w], op=ALU.add),
                 reads=R(tqm[0], tqm[1]), writes=R(tqm[0]))
            p.op("pool", lambda e, tqm=tqm, m=m, w=w: e.tensor_tensor(out=mrg[:, m, :w], in0=tqm[0][:, :w], in1=tqm[2][:, :w], op=ALU.add),
                 reads=R(tqm[0], tqm[2]), writes=R(mrg))
        h1 = ht
        for m in range(8):
            b = nextbank()
            emit_proj(cx, b, wout, m * 128, 128, mrg, w)
            p.op("dve", lambda e, b=b, h1=h1, ht=ht, m=m, w=w: e.tensor_tensor(
                out=h1[:, m, :w], in0=b[:, :w], in1=ht[:, m, :w], op=ALU.add), reads=R(b, ht), writes=R(h1))
        p.dma("sp", lambda e, h1=h1, t0=t0, w=w: e.dma_start(out=fm(H1a)[:, :, t0:t0 + w], in_=h1[:, :, :w]), reads=R(h1), writes=[(H1, ("w", t0))])
        if kind == "dense":
            hn = hnb[0]
            emit_rmsnorm(cx, h1, w, gffn, ones_bf, sq, bk[0], lnv, rstd, hn)
            p.dma("sp", lambda e, hn=hn, t0=t0, w=w: e.dma_start(out=fm(HNa)[:, :, t0:t0 + w], in_=hn[:, :, :w]), reads=R(hn), writes=[(HN, ("w", t0))])
    cx.reset(mark0)

    if kind == "dense":
        NF = 22
        NPASS = 2
        FP = NF // NPASS
        fg = cx.sb("fg", [128, 8, FP * 128], BF16)
        fu = cx.sb("fu", [128, 8, FP * 128], BF16)
        fd = cx.sb("fd", [128, FP, 1024], BF16)
        hb = [cx.sb(f"hb{i}", [128, 8, 512]) for i in range(2)]
        hnb2 = [cx.sb(f"hnb{i}", [128, 8, 512], BF16) for i in range(2)]
        ab = [cx.sb(f"ab{i}", [128, FP, 512], BF16) for i in range(2)]
        sgs = [cx.sb(f"sgs{i}", [128, 512], BF16) for i in range(2)]
        tc = 0
        for ps_ in range(NPASS):
            f0 = ps_ * FP
            for c in range(KC):
                p.dma("pool", lambda e, c=c, f0=f0: e.dma_start(out=fg[:, c, :], in_=fg_d[c * 128:(c + 1) * 128, f0 * 128:(f0 + FP) * 128]), writes=R(fg))
                p.dma("pool", lambda e, c=c, f0=f0: e.dma_start(out=fu[:, c, :], in_=fu_d[c * 128:(c + 1) * 128, f0 * 128:(f0 + FP) * 128]), writes=R(fu))
            for f in range(FP):
                p.dma("pool", lambda e, f=f, f0=f0: e.dma_start(out=fd[:, f, :], in_=fd_d[(f0 + f) * 128:(f0 + f + 1) * 128, :]), writes=R(fd))
            for i, (t0, w) in enumerate(tiles):
                h_ = hb[tc % 2]
                hn = hnb2[tc % 2]
                a_ = ab[tc % 2]
                p.dma("sp", lambda e, h_=h_, t0=t0, w=w: e.dma_start(out=h_[:, :, :w], in_=fm(H1a)[:, :, t0:t0 + w]), reads=R(H1), writes=R(h_))
                p.dma("sp", lambda e, hn=hn, t0=t0, w=w: e.dma_start(out=hn[:, :, :w], in_=fm(HNa)[:, :, t0:t0 + w]), reads=R(HN), writes=R(hn))
                for f in range(FP):
                    bg_ = nextbank()
                    emit_proj(cx, bg_, fg, f * 128, 128, hn, w)
                    bu_ = nextbank()
                    emit_proj(cx, bu_, fu, f * 128, 128, hn, w)
                    sg = sgs[f % 2]
                    p.op("act", lambda e, sg=sg, bg_=bg_, w=w: e.activation(out=sg[:, :w], in_=bg_[:, :w], func=AF.Silu), reads=R(bg_), writes=R(sg))
                    p.op("dve", lambda e, sg=sg, bu_=bu_, a_=a_, f=f, w=w: e.tensor_tensor(
                        out=a_[:, f, :w], in0=bu_[:, :w], in1=sg[:, :w], op=ALU.mult), reads=R(bu_, sg), writes=R(a_))
                for m in range(8):
                    b = nextbank()
                    for f in range(FP):
                        p.op("pe", lambda e, b=b, f=f, m=m, a_=a_, w=w: e.matmul(
                            b[:, :w], fd[:, f, m * 128:(m + 1) * 128], a_[:, f, :w], start=(f == 0), stop=(f == FP - 1)),
                            reads=R(fd, a_), writes=R(b))
                    p.op("dve", lambda e, b=b, h_=h_, m=m, w=w: e.tensor_tensor(
                        out=h_[:, m, :w], in0=b[:, :w], in1=h_[:, m, :w], op=ALU.add), reads=R(b, h_), writes=R(h_))
                if ps_ == NPASS - 1:
                    p.dma("sp", lambda e, h_=h_, t0=t0, w=w: e.dma_start(out=fm(hout)[:, :, t0:t0 + w], in_=h_[:, :, :w]), reads=R(h_), writes=[(hout_t, ("w", t0))])
                else:
                    p.dma("sp", lambda e, h_=h_, t0=t0, w=w: e.dma_start(out=fm(H1a)[:, :, t0:t0 + w], in_=h_[:, :, :w]), reads=R(h_), writes=R(H1))
                tc += 1
    else:
        emit_moe(cx, Tn, tiles, H1, gffn, ones_bf, sq, lnv, rstd, rw_d, mg_d, mu_d, md_d, gfin_d, ident_d, sel_d,
                 hout, hout_t, n_exp, dff_e, nextbank)


def build_B(Tn, kind, n_exp=8, dff_e=3584):
    from contextlib import ExitStack
    nc = bass.Bass("TRN2", target_bir_lowering=False)

    def din(name, shape, dt=F32):
        return nc.dram_tensor(name, shape, dt, kind="ExternalInput").ap()

    hT = din("hT", [D, Tn])
    attT = din("attT", [1536, Tn], BF16)
    specs = B_IN_COMMON + (B_IN_DENSE if kind == "dense" else b_in_moe(n_exp, dff_e))
    I = {k: din(k, shp) for k, shp in specs}
    hout = nc.dram_tensor("hout", [D, Tn], F32, kind="ExternalOutput").ap()
    with ExitStack() as st:
        cx = Ctx(nc, st)
        ones_bf = cx.sb("ones_bf", [128, 128], BF16)
        cx.p.op("pool", lambda e: e.memset(ones_bf[:], 1.0), writes=R(ones_bf))
        S = {"ones_bf": ones_bf, "mark0": cx.mark()}
        stage_B(cx, Tn, kind, I, hT, TT(hT, "hT"), attT, TT(attT, "attT"), hout, TT(hout, "hout"), S, n_exp=n_exp, dff_e=dff_e)
        cx.p.final_wait("sp")
        cx.p.build(st)
    return nc


def prep_B(layer, hT_loc, attT_loc, P, kind, n_exp=8):
    f = np.ascontiguousarray
    w_in = P["w_in"][layer]
    bgate = P["b_gate"][layer]
    ins = {
        "hT": (None if hT_loc is None else f(hT_loc)), "attT": (None if attT_loc is None else f(attT_loc)), "gmix": _pc(P["g_mix"][layer]), "gffn": _pc(P["g_ffn"][layer]),
        "w_g": f(w_in[:, 4100:4100 + 3072]), "b_g": f(bgate.reshape(24, 128).T),
        "w_o3": f(np.concatenate([P["w_fox_o"][layer], P["w_sb_o"][layer], P["w_lru_o"][layer]], axis=0)),
        "w_out": f(P["w_out"][layer]),
    }
    if kind == "dense":
        ins["ffn_wg"] = f(P["ffn_w_gate"][0])
        ins["ffn_wu"] = f(P["ffn_w_up"][0])
        ins["ffn_wd"] = f(P["ffn_w_down"][0])
    else:
        ins["router_w"] = f(P["router_w"][0].reshape(8, 128, 8).transpose(1, 0, 2))
        ins["moe_wg"] = f(P["moe_w_gate"][0][:n_exp])
        ins["moe_wu"] = f(P["moe_w_up"][0][:n_exp])
        ins["moe_wd"] = f(P["moe_w_down"][0][:n_exp])
        ins["gfin"] = _pc(P["g_final"])
        ins["ident"] = np.eye(128, dtype=np.float32)
        sel = np.zeros((8, 8, 128), np.float32)
        for e in range(8):
            sel[e, e, :] = 1.0
        ins["sel8"] = sel
    return ins


def emit_moe(cx, Tn, tiles, H1, gffn, ones_bf, sq, lnv, rstd, rw_d, mg_d, mu_d, md_d, gfin_d, ident_d, sel_d,
             hout, hout_t, n_exp, dff_e, nextbank):
    p = cx.p
    bk = cx.banks
    NG = dff_e // 512
    H1a = H1.h.ap()

    def fm(ap_):
        return ap_.rearrange("(c p) t -> p c t", p=128)

    nt = len(tiles)
    half_n = nt // 2
    sts = [tiles[:half_n], tiles[half_n:]] if nt > 1 else [tiles]
    TS = max(sum(w for _, w in s_) for s_ in sts)
    y = cx.sb("y", [128, 8, TS])
    hn = cx.sb("hn", [128, 8, TS], BF16)
    wgb = [cx.sb(f"wgb{i}", [128, 8, 512], BF16) for i in range(2)]
    wub = [cx.sb(f"wub{i}", [128, 8, 512], BF16) for i in range(2)]
    wdb = [cx.sb(f"wdb{i}", [128, 4, 1024], BF16) for i in range(2)]
    combT = cx.sb("combT", [8, TS])
    combB = cx.sb("combB", [128, TS])
    sgs = [cx.sb(f"sgs{i}", [128, 512], BF16) for i in range(2)]
    tmp = [cx.sb(f"tmp{i}", [128, 512]) for i in range(2)]
    xn32 = cx.sb("xn32", [128, 8, 128])
    rw = cx.sb("rw", [128, 8, 8])
    ident = cx.sb("ident", [128, 128])
    sel = cx.sb("sel", [8, 8, 128])
    gfin = cx.sb("gfin", [128, 8])
    lg = cx.sb("lg", [128, 8])
    l2 = cx.sb("l2", [128, 8])
    eq1 = cx.sb("eq1", [128, 8])
    eq2 = cx.sb("eq2", [128, 8])
    cmb = cx.sb("cmb", [128, 8])
    m1 = cx.sb("m1", [128, 1])
    m2 = cx.sb("m2", [128, 1])
    dd = cx.sb("dd", [128, 1])
    w1 = cx.sb("w1", [128, 1])
    w2 = cx.sb("w2", [128, 1])
    p.dma("sp", lambda e: e.dma_start(out=rw[:], in_=rw_d), writes=R(rw))
    p.dma("sp", lambda e: e.dma_start(out=ident[:], in_=ident_d), writes=R(ident))
    p.dma("sp", lambda e: e.dma_start(out=sel[:], in_=sel_d), writes=R(sel))
    p.dma("sp", lambda e: e.dma_start(out=gfin[:], in_=gfin_d), writes=R(gfin))
    AX = mybir.AxisListType.X
    wcount = 0
    for si, stl in enumerate(sts):
        base = stl[0][0]
        for j, (t0, w) in enumerate(stl):
            c0 = t0 - base
            p.dma("sp", lambda e, c0=c0, t0=t0, w=w: e.dma_start(out=y[:, :, c0:c0 + w], in_=fm(H1a)[:, :, t0:t0 + w]),
                  reads=R(H1), writes=[(y, j)])
            p.op("act", lambda e, c0=c0, w=w: e.activation(out=sq[:, :, :w], in_=y[:, :, c0:c0 + w], func=AF.Square),
                 reads=[(y, j)], writes=[(sq, "lo"), (sq, "hi")])
            for c in range(KC):
                p.op("pe", lambda e, c=c, w=w: e.matmul(bk[0][:, :w], ones_bf[:, :], sq[:, c, :w], start=(c == 0), stop=(c == KC - 1)),
                     reads=R(ones_bf) + [(sq, "lo"), (sq, "hi")], writes=R(bk[0]))
            p.op("act", lambda e, w=w: e.activation(out=lnv[:, :w], in_=bk[0][:, :w], func=AF.Ln, scale=1.0 / D, bias=RMS_EPS),
                 reads=R(bk[0]), writes=R(lnv))
            p.op("act", lambda e, w=w: e.activation(out=rstd[:, :w], in_=lnv[:, :w], func=AF.Exp, scale=-0.5), reads=R(lnv), writes=R(rstd))
            for c in range(KC):
                p.op("dve", lambda e, c=c, c0=c0, w=w: e.scalar_tensor_tensor(
                    out=hn[:, c, c0:c0 + w], in0=y[:, c, c0:c0 + w], scalar=gffn[:, c:c + 1], in1=rstd[:, :w], op0=ALU.mult, op1=ALU.mult),
                    reads=[(y, j)] + R(gffn, rstd), writes=[(hn, j)])
            for s in range((w + 127) // 128):
                ws = min(128, w - s * 128)
                cs = c0 + s * 128
                for c in range(KC):
                    p.op("dve", lambda e, c=c, cs=cs, ws=ws, s=s: e.scalar_tensor_tensor(
                        out=xn32[:, c, :ws], in0=y[:, c, cs:cs + ws], scalar=gffn[:, c:c + 1], in1=rstd[:, s * 128:s * 128 + ws],
                        op0=ALU.mult, op1=ALU.mult), reads=[(y, j)] + R(gffn, rstd), writes=R(xn32))
                lb = nextbank()
                for c in range(KC):
                    p.op("pe", lambda e, c=c, lb=lb, ws=ws: e.matmul(lb[:ws, 0:8], xn32[:, c, :ws], rw[:, c, :], start=(c == 0), stop=(c == KC - 1)),
                         reads=R(xn32, rw), writes=R(lb))
                p.op("dve", lambda e, lb=lb, ws=ws: e.tensor_copy(out=lg[:ws, :], in_=lb[:ws, 0:8]), reads=R(lb), writes=R(lg))
                p.op("dve", lambda e, ws=ws: e.tensor_reduce(out=m1[:ws, :], in_=lg[:ws, :], axis=AX, op=ALU.max), reads=R(lg), writes=R(m1))
                p.op("dve", lambda e, ws=ws: e.tensor_scalar(out=eq1[:ws, :], in0=lg[:ws, :], scalar1=m1[:ws, 0:1], scalar2=None, op0=ALU.is_equal),
                     reads=R(lg, m1), writes=R(eq1))
                p.op("dve", lambda e, ws=ws: e.scalar_tensor_tensor(out=l2[:ws, :], in0=eq1[:ws, :], scalar=-1e30, in1=lg[:ws, :], op0=ALU.mult, op1=ALU.add),
                     reads=R(eq1, lg), writes=R(l2))
                p.op("dve", lambda e, ws=ws: e.tensor_reduce(out=m2[:ws, :], in_=l2[:ws, :], axis=AX, op=ALU.max), reads=R(l2), writes=R(m2))
                p.op("dve", lambda e, ws=ws: e.tensor_scalar(out=eq2[:ws, :], in0=l2[:ws, :], scalar1=m2[:ws, 0:1], scalar2=None, op0=ALU.is_equal),
                     reads=R(l2, m2), writes=R(eq2))
                p.op("dve", lambda e, ws=ws: e.tensor_tensor(out=dd[:ws, :], in0=m1[:ws, :], in1=m2[:ws, :], op=ALU.subtract), reads=R(m1, m2), writes=R(dd))
                p.op("act", lambda e, ws=ws: e.activation(out=w1[:ws, :], in_=dd[:ws, :], func=AF.Sigmoid), reads=R(dd), writes=R(w1))
                p.op("act", lambda e, ws=ws: e.activation(out=w2[:ws, :], in_=dd[:ws, :], func=AF.Sigmoid, scale=-1.0), reads=R(dd), writes=R(w2))
                p.op("dve", lambda e, ws=ws: e.tensor_scalar(out=cmb[:ws, :], in0=eq1[:ws, :], scalar1=w1[:ws, 0:1], scalar2=None, op0=ALU.mult),
                     reads=R(eq1, w1), writes=R(cmb))
                p.op("dve", lambda e, ws=ws: e.scalar_tensor_tensor(out=cmb[:ws, :], in0=eq2[:ws, :], scalar=w2[:ws, 0:1], in1=cmb[:ws, :], op0=ALU.mult, op1=ALU.add),
                     reads=R(eq2, w2, cmb), writes=R(cmb))
                tb = nextbank()
                p.op("pe", lambda e, tb=tb, ws=ws: e.transpose(tb[0:8, :ws], cmb[:ws, :], ident[:ws, :ws]), reads=R(cmb, ident), writes=R(tb))
                p.op("dve", lambda e, tb=tb, cs=cs, ws=ws: e.tensor_copy(out=combT[:, cs:cs + ws], in_=tb[0:8, :ws]), reads=R(tb), writes=[(combT, j)])
        groups = [(ex, g) for ex in range(n_exp) for g in range(NG)]

        def load_group(gi):
            ex, g = groups[gi]
            k = (wbase + gi) % 2
            for c in range(KC):
                p.dma("pool", lambda e, c=c, k=k, ex=ex, g=g: e.dma_start(
                    out=wgb[k][:, c, :], in_=mg_d[ex, c * 128:(c + 1) * 128, g * 512:(g + 1) * 512]), writes=R(wgb[k]))
                p.dma("pool", lambda e, c=c, k=k, ex=ex, g=g: e.dma_start(
                    out=wub[k][:, c, :], in_=mu_d[ex, c * 128:(c + 1) * 128, g * 512:(g + 1) * 512]), writes=R(wub[k]))
            for f in range(4):
                p.dma("pool", lambda e, f=f, k=k, ex=ex, g=g: e.dma_start(
                    out=wdb[k][:, f, :], in_=md_d[ex, g * 512 + f * 128:g * 512 + (f + 1) * 128, :]), writes=R(wdb[k]))

        wbase = wcount
        load_group(0)
        for gi, (ex, g) in enumerate(groups):
            if True:
                k = (wbase + gi) % 2
                wcount += 1
                if gi + 1 < len(groups):
                    load_group(gi + 1)
                for j, (t0, w) in enumerate(stl):
                    c0 = t0 - base
                    if g == 0:
                        cbk = nextbank()
                        p.op("pe", lambda e, cbk=cbk, ex=ex, c0=c0, w=w: e.matmul(cbk[:, :w], sel[:, ex, :], combT[:, c0:c0 + w], start=True, stop=True),
                             reads=R(sel) + [(combT, j)], writes=R(cbk))
                        p.op("act", lambda e, cbk=cbk, c0=c0, w=w: e.activation(out=combB[:, c0:c0 + w], in_=cbk[:, :w], func=AF.Copy),
                             reads=R(cbk), writes=[(combB, j)])
                    akey = "lo" if j % 2 == 0 else "hi"
                    aoff = 0 if j % 2 == 0 else 4
                    for f in range(4):
                        bg_ = nextbank()
                        for c in range(KC):
                            p.op("pe", lambda e, bg_=bg_, c=c, f=f, k=k, c0=c0, w=w: e.matmul(
                                bg_[:, :w], wgb[k][:, c, f * 128:(f + 1) * 128], hn[:, c, c0:c0 + w], start=(c == 0), stop=(c == KC - 1)),
                                reads=R(wgb[k]) + [(hn, j)], writes=R(bg_))
                        bu_ = nextbank()
                        for c in range(KC):
                            p.op("pe", lambda e, bu_=bu_, c=c, f=f, k=k, c0=c0, w=w: e.matmul(
                                bu_[:, :w], wub[k][:, c, f * 128:(f + 1) * 128], hn[:, c, c0:c0 + w], start=(c == 0), stop=(c == KC - 1)),
                                reads=R(wub[k]) + [(hn, j)], writes=R(bu_))
                        sg = sgs[f % 2]
                        p.op("act", lambda e, sg=sg, bg_=bg_, w=w: e.activation(out=sg[:, :w], in_=bg_[:, :w], func=AF.Silu), reads=R(bg_), writes=R(sg))
                        p.op("dve", lambda e, sg=sg, bu_=bu_, f=f, aoff=aoff, w=w: e.tensor_tensor(
                            out=sq[:, aoff + f, :w], in0=bu_[:, :w], in1=sg[:, :w], op=ALU.mult), reads=R(bu_, sg), writes=[(sq, akey)])
                    for m in range(8):
                        b = nextbank()
                        for f in range(4):
                            p.op("pe", lambda e, b=b, f=f, m=m, k=k, aoff=aoff, w=w: e.matmul(
                                b[:, :w], wdb[k][:, f, m * 128:(m + 1) * 128], sq[:, aoff + f, :w], start=(f == 0), stop=(f == 3)),
                                reads=R(wdb[k]) + [(sq, akey)], writes=R(b))
                        t_ = tmp[m % 2]
                        p.op("dve", lambda e, b=b, t_=t_, c0=c0, w=w: e.tensor_tensor(out=t_[:, :w], in0=b[:, :w], in1=combB[:, c0:c0 + w], op=ALU.mult),
                             reads=R(b) + [(combB, j)], writes=R(t_))
                        p.op("dve", lambda e, t_=t_, m=m, c0=c0, w=w: e.tensor_tensor(out=y[:, m, c0:c0 + w], in0=y[:, m, c0:c0 + w], in1=t_[:, :w], op=ALU.add),
                             reads=R(t_) + [(y, j)], writes=[(y, j)])
        for j, (t0, w) in enumerate(stl):
            c0 = t0 - base
            p.op("act", lambda e, c0=c0, w=w: e.activation(out=sq[:, :, :w], in_=y[:, :, c0:c0 + w], func=AF.Square),
                 reads=[(y, j)], writes=[(sq, "lo"), (sq, "hi")])
            for c in range(KC):
                p.op("pe", lambda e, c=c, w=w: e.matmul(bk[0][:, :w], ones_bf[:, :], sq[:, c, :w], start=(c == 0), stop=(c == KC - 1)),
                     reads=R(ones_bf) + [(sq, "lo"), (sq, "hi")], writes=R(bk[0]))
            p.op("act", lambda e, w=w: e.activation(out=lnv[:, :w], in_=bk[0][:, :w], func=AF.Ln, scale=1.0 / D, bias=RMS_EPS),
                 reads=R(bk[0]), writes=R(lnv))
            p.op("act", lambda e, w=w: e.activation(out=rstd[:, :w], in_=lnv[:, :w], func=AF.Exp, scale=-0.5), reads=R(lnv), writes=R(rstd))
            for c in range(KC):
                p.op("dve", lambda e, c=c, c0=c0, w=w: e.scalar_tensor_tensor(
                    out=y[:, c, c0:c0 + w], in0=y[:, c, c0:c0 + w], scalar=gfin[:, c:c + 1], in1=rstd[:, :w], op0=ALU.mult, op1=ALU.mult),
                    reads=[(y, j)] + R(gfin, rstd), writes=[(y, j)])
            p.dma("sp", lambda e, c0=c0, t0=t0, w=w: e.dma_start(out=fm(hout)[:, :, t0:t0 + w], in_=y[:, :, c0:c0 + w]), reads=[(y, j)], writes=[(hout_t, ("w", t0))])


def stage_blend(cx, Lp, Tn, sel_d, Hs, At, hmy, amy, S):
    p = cx.p
    cx.reset(S["mark0"])
    sel = cx.sb("sel2", [128, 2])
    p.dma("sp", lambda e: e.dma_start(out=sel[:], in_=sel_d), writes=R(sel))
    ha = cx.sb("bl_ha", [128, 8, 512])
    hb = cx.sb("bl_hb", [128, 8, 512])
    aa = cx.sb("bl_aa", [128, 12, 512], BF16)
    ab = cx.sb("bl_ab", [128, 12, 512], BF16)
    Hsa, Ata, hmya, amya = Hs.h.ap(), At.h.ap(), hmy.h.ap(), amy.h.ap()

    def fm(ap_):
        return ap_.rearrange("(c p) t -> p c t", p=128)

    for (t0, w) in [(t0, min(512, Tn - t0)) for t0 in range(0, Tn, 512)]:
        for (xa, xb, src, dst, dstt) in ((ha, hb, Hsa, hmya, hmy), (aa, ab, Ata, amya, amy)):
            p.dma("sp", lambda e, xa=xa, src=src, t0=t0, w=w: e.dma_start(out=xa[:, :, :w], in_=fm(src)[:, :, t0:t0 + w]), writes=R(xa))
            p.dma("sp", lambda e, xb=xb, src=src, t0=t0, w=w: e.dma_start(out=xb[:, :, :w], in_=fm(src)[:, :, Tn + t0:Tn + t0 + w]), writes=R(xb))
            p.op("dve", lambda e, xa=xa, w=w: e.tensor_scalar(out=xa[:, :, :w], in0=xa[:, :, :w], scalar1=sel[:, 0:1], scalar2=None, op0=ALU.mult),
                 reads=R(xa, sel), writes=R(xa))
            p.op("dve", lambda e, xa=xa, xb=xb, w=w: e.scalar_tensor_tensor(out=xa[:, :, :w], in0=xb[:, :, :w], scalar=sel[:, 1:2], in1=xa[:, :, :w],
                                                                           op0=ALU.mult, op1=ALU.add), reads=R(xa, xb, sel), writes=R(xa))
            p.dma("sp", lambda e, xa=xa, dst=dst, t0=t0, w=w: e.dma_start(out=fm(dst)[:, :, t0:t0 + w], in_=xa[:, :, :w]),
                  reads=R(xa), writes=[(dstt, ("w", t0))])


def build_F(Lp, n_exp=8, dff_e=3584):
    from contextlib import ExitStack
    nc = bass.Bass("TRN2", target_bir_lowering=False)
    Tn = Lp // 2

    def din(name, shape, dt=F32):
        return nc.dram_tensor(name, shape, dt, kind="ExternalInput").ap()

    hT0 = din("hT", [D, Lp])
    mle_d = din("mask_le", [128, 4, 512])
    mlt_d = din("mask_lt", [128, 4, 512])
    negu_d = din("negU", [128, 128])
    sel_d = din("sel2", [128, 2])
    IA = [[{k: din(f"a{l}{h}_{k}", shp) for k, shp in A_IN} for h in range(2)] for l in range(2)]
    IB0 = {k: din("b0_" + k, shp) for k, shp in B_IN_COMMON + B_IN_DENSE}
    IB1 = {k: din("b1_" + k, shp) for k, shp in B_IN_COMMON + b_in_moe(n_exp, dff_e)}
    hout = nc.dram_tensor("hout", [D, Tn], F32, kind="ExternalOutput").ap()
    with ExitStack() as st:
        cx = Ctx(nc, st)
        S = load_consts(cx, mle_d, mlt_d, negu_d, Lp)
        att0 = cx.dram("att0s", [1536, Lp], BF16)
        att1 = cx.dram("att1s", [1536, Lp], BF16)
        Hs1 = cx.dram("Hs1s", [D, Lp], F32)
        hmy = cx.dram("hmys", [D, Tn], F32)
        amy = cx.dram("amys", [1536, Tn], BF16)
        hT0_t = TT(hT0, "hT0")
        rows = ((0, 512, 1024), (256, 768, 1280))
        for h in range(2):
            stage_A(cx, Lp, IA[0][h], hT0, hT0_t, att0.h.ap(), att0, rows[h], S)
        stage_B(cx, Lp, "dense", IB0, hT0, hT0_t, att0.h.ap(), att0, Hs1.h.ap(), Hs1, S, tag="L0")
        for h in range(2):
            stage_A(cx, Lp, IA[1][h], Hs1.h.ap(), Hs1, att1.h.ap(), att1, rows[h], S)
        stage_blend(cx, Lp, Tn, sel_d, Hs1, att1, hmy, amy, S)
        stage_B(cx, Tn, "moe", IB1, hmy.h.ap(), hmy, amy.h.ap(), amy, hout, TT(hout, "hout"), S, tag="L1", n_exp=n_exp, dff_e=dff_e)
        cx.p.final_wait("sp")
        cx.p.build(st)
    return nc


def prep_F(r, hT_b, P, n_exp=8):
    ins = {"hT": np.ascontiguousarray(hT_b)}
    mle, mlt, negu = _consts()
    ins.update(mask_le=mle, mask_lt=mlt, negU=negu)
    sel = np.zeros((128, 2), np.float32)
    sel[:, r] = 1.0
    ins["sel2"] = sel
    for l in range(2):
        for h in range(2):
            half = r if h == 0 else 1 - r
            d = prep_A(l, half, None, P)
            for k, _ in A_IN:
                ins[f"a{l}{h}_{k}"] = d[k]
    order = np.concatenate([np.arange(br * 512 + hf * 256, br * 512 + hf * 256 + 256) for br in range(3) for hf in (r, 1 - r)])
    for l, kind in ((0, "dense"), (1, "moe")):
        d = prep_B(l, None, None, P, kind, n_exp=n_exp)
        d["w_o3"] = np.ascontiguousarray(d["w_o3"][order])
        for k, v in d.items():
            if k in ("hT", "attT"):
                continue
            ins[f"b{l}_{k}"] = v
    return ins


def kernel_fused(**inputs):
    import sys
    import time
    P = {k: np.asarray(v) for k, v in inputs.items()}
    x = P["x"]
    B, S_, _ = x.shape
    L = S_ + N_META
    Lp = (L + 127) // 128 * 128
    if (Lp // 2) % 64 != 0:
        Lp += 128
    Tn = Lp // 2
    hT = np.zeros((B, D, Lp), np.float32)
    hT[:, :, :N_META] = P["meta_tokens"].T[None]
    hT[:, :, N_META:L] = x.transpose(0, 2, 1)
    cores = list(range(8))
    t0 = time.time()
    ncF = _get_nc(("F", Lp), lambda: build_F(Lp))
    print(f"[kernel] build {time.time() - t0:.1f}s", file=sys.stderr, flush=True)
    in_maps = [prep_F(c % 2, hT[c // 2], P) for c in cores]
    t0 = time.time()
    res = run_bass_kernel_spmd(ncF, in_maps, core_ids=cores).results
    print(f"[kernel] fused launch done {time.time() - t0:.1f}s", file=sys.stderr, flush=True)
    full = np.stack([np.concatenate([res[2 * b]["hout"], res[2 * b + 1]["hout"]], axis=1) for b in range(B)])
    return np.ascontiguousarray(full[:, :, N_META:L].transpose(0, 2, 1)).astype(np.float32)


_NC_CACHE = {}
N_META = 16


def _get_nc(key, builder):
    if key not in _NC_CACHE:
        _NC_CACHE[key] = builder()
    return _NC_CACHE[key]


def kernel_unfused(**inputs):
    import sys
    import time
    P = {k: np.asarray(v) for k, v in inputs.items()}
    x = P["x"]
    B, S, _ = x.shape
    L = S + N_META
    Lp = (L + 127) // 128 * 128
    if (Lp // 2) % 64 != 0:
        Lp += 128
    Tn = Lp // 2
    hT = np.zeros((B, D, Lp), np.float32)
    hT[:, :, :N_META] = P["meta_tokens"].T[None]
    hT[:, :, N_META:L] = x.transpose(0, 2, 1)
    cores = list(range(8))
    for layer in range(2):
        t0 = time.time()
        ncA = _get_nc(("A", Lp), lambda: build_A(Lp))
        in_maps = [prep_A(layer, c % 2, hT[c // 2], P) for c in cores]
        resA = run_bass_kernel_spmd(ncA, in_maps, core_ids=cores).results
        print(f"[kernel] layer {layer} A done {time.time() - t0:.1f}s", file=sys.stderr, flush=True)
        t0 = time.time()
        kind = "dense" if layer == 0 else "moe"
        ncB = _get_nc(("B", Tn, kind), lambda: build_B(Tn, kind))
        in_maps = []
        for c in cores:
            b, half = c // 2, c % 2
            a0 = resA[2 * b]["attT"]
            a1 = resA[2 * b + 1]["attT"]
            sl = slice(half * Tn, (half + 1) * Tn)
            att = np.concatenate([a0[0:256, sl], a1[0:256, sl], a0[256:512, sl], a1[256:512, sl],
                                  a0[512:768, sl], a1[512:768, sl]], axis=0)
            in_maps.append(prep_B(layer, hT[b][:, sl], att, P, kind))
        del resA
        resB = run_bass_kernel_spmd(ncB, in_maps, core_ids=cores).results
        del in_maps
        print(f"[kernel] layer {layer} B done {time.time() - t0:.1f}s", file=sys.stderr, flush=True)
        hT = np.stack([np.concatenate([resB[2 * b]["hout"], resB[2 * b + 1]["hout"]], axis=1) for b in range(B)])
        del resB
    out = np.ascontiguousarray(hT[:, :, N_META:L].transpose(0, 2, 1)).astype(np.float32)
    return out


FUSED = True


def kernel(**inputs):
    if FUSED:
        return kernel_fused(**inputs)
    return kernel_unfused(**inputs)
```
